# Optimizing a Trainium2 kernel written in Bass

```python
import math
import jax
import jax.numpy as jnp
from jax import lax
import numpy as np

D_MODEL = 1024
BATCH = 8
SEQ = 2048
DEPTH = 4

GRID_W = 64
CTX_LEN = 256
N_EVEN = (DEPTH + 1) // 2
N_ODD = DEPTH // 2
NORM_EPS = 1e-6
ROPE_THETA = 10000.0
N_MOD = 6

ML_HEADS = 4
ML_DQK = 64
ML_DV = 128
ML_CHUNK = 64
ML_CONV = 3
ML_W = ML_HEADS * ML_DV

DA_HEADS = 4
DA_DQK = 64
DA_DV = 128
DA_W = DA_HEADS * DA_DV
Q_BLOCK = 128

EVEN_SIZES = (2 * ML_HEADS * ML_DQK, ML_W, ML_W, 4 * ML_HEADS,
              DA_HEADS * 2 * DA_DQK, DA_HEADS * 2 * DA_DQK, DA_W)
EVEN_IN = sum(EVEN_SIZES)
EVEN_SPLITS = tuple(sum(EVEN_SIZES[:i + 1]) for i in range(len(EVEN_SIZES) - 1))
MIX_W = ML_W + DA_W

WA_HEADS = 16
WA_KV_HEADS = 4
WA_GROUP = WA_HEADS // WA_KV_HEADS
WA_DH = 64
WINDOW = 128
W_BLOCK = WINDOW
WA_W = WA_HEADS * WA_DH
WA_KV_W = WA_KV_HEADS * WA_DH
ODD_IN = WA_W + 2 * WA_KV_W
ODD_SPLITS = (WA_W, WA_W + WA_KV_W)

PEER_HEADS = 8
PEER_NKEYS = 128
PEER_EXPERTS = PEER_NKEYS * PEER_NKEYS
PEER_DQ = 256
PEER_DHALF = PEER_DQ // 2
PEER_TOPK = 16
PEER_TOK_BLOCK = 128

kernel_name = 'hybrid_mlstm_diffattn_swa_peer_dit'


def _rmsnorm(x, g):
    xf = x.astype(jnp.float32)
    y = xf * lax.rsqrt(jnp.mean(xf * xf, axis=-1, keepdims=True) + NORM_EPS)
    return (y * g.astype(jnp.float32)).astype(x.dtype)


def _headnorm(x, g):
    xf = x.astype(jnp.float32)
    y = xf * lax.rsqrt(jnp.mean(xf * xf, axis=-1, keepdims=True) + NORM_EPS)
    y = y * g.astype(jnp.float32).reshape(x.shape[-2:])
    return y.reshape(x.shape[:-2] + (-1,))


def _axial_rope_tables(n_tok, dim):
    rows = n_tok // GRID_W
    r = jnp.repeat(jnp.arange(rows, dtype=jnp.float32), GRID_W)
    col = jnp.broadcast_to(jnp.arange(GRID_W, dtype=jnp.float32), (rows, GRID_W)).reshape(-1)
    nf = dim // 4
    inv = ROPE_THETA ** (-jnp.arange(nf, dtype=jnp.float32) / nf)
    ang = jnp.concatenate([r[:, None] * inv, col[:, None] * inv], axis=-1)
    return jnp.cos(ang), jnp.sin(ang)


def _apply_rope(x, cos, sin):
    shp = (x.shape[1],) + (1,) * (x.ndim - 3) + (cos.shape[-1],)
    cs = cos.reshape(shp)
    sn = sin.reshape(shp)
    xf = x.astype(jnp.float32)
    x1, x2 = jnp.split(xf, 2, axis=-1)
    return jnp.concatenate([x1 * cs - x2 * sn, x1 * sn + x2 * cs], axis=-1).astype(x.dtype)


def _centred_conv(x, w, b):
    taps = w.shape[0]
    p = taps // 2
    t = x.shape[1]
    xp = jnp.pad(x, ((0, 0), (p, p), (0, 0)))
    out = b
    for j in range(taps):
        out = out + xp[:, j:j + t] * w[j]
    return out


def _mlstm_prepare(qk, v, g, conv_w, conv_b, gate_b):
    bsz, t, _ = qk.shape
    qk = jax.nn.silu(_centred_conv(qk, conv_w, conv_b))
    q, k = jnp.split(qk, 2, axis=-1)

    def heads(a, d):
        return a.reshape(bsz, t, ML_HEADS, d).transpose(0, 2, 1, 3).astype(jnp.float32)

    q = heads(q, ML_DQK)
    k = heads(k, ML_DQK) * (ML_DQK ** -0.5)
    v = heads(v, ML_DV)
    g = (g + gate_b).astype(jnp.float32).reshape(bsz, t, 4, ML_HEADS).transpose(2, 0, 3, 1)
    return (q, k, v, g[0], jax.nn.log_sigmoid(g[1]), g[2], jax.nn.log_sigmoid(g[3]))


def _mlstm_state0(bsz):
    return (jnp.zeros((bsz, ML_HEADS, ML_DV, ML_DQK), jnp.float32),
            jnp.zeros((bsz, ML_HEADS, ML_DQK), jnp.float32),
            jnp.zeros((bsz, ML_HEADS), jnp.float32))


def _mlstm_chunk_scan(q, k, v, ig, lf, state):
    bsz, nh, t, _ = q.shape
    nc = t // ML_CHUNK

    def to_chunks(a):
        return jnp.moveaxis(a.reshape((bsz, nh, nc, ML_CHUNK) + a.shape[3:]), 2, 0)

    tri = jnp.tril(jnp.ones((ML_CHUNK, ML_CHUNK), dtype=bool))

    def step(carry, xs):
        c_st, n_st, m_st = carry
        qc, kc, vc, ic, fc = xs
        b = jnp.cumsum(fc, axis=-1)
        log_d = jnp.where(tri, b[..., :, None] - b[..., None, :] + ic[..., None, :], -jnp.inf)
        inter = b + m_st[..., None]
        m_t = jnp.maximum(inter, jnp.max(log_d, axis=-1))
        s = jnp.einsum('bhtd,bhsd->bhts', qc, kc) * jnp.exp(log_d - m_t[..., None])
        w_inter = jnp.exp(inter - m_t)
        num = (jnp.einsum('bhts,bhsv->bhtv', s, vc)
               + w_inter[..., None] * jnp.einsum('bhvd,bhtd->bhtv', c_st, qc))
        den = jnp.sum(s, axis=-1) + w_inter * jnp.einsum('bhd,bhtd->bht', n_st, qc)
        h = num / jnp.maximum(jnp.abs(den), jnp.exp(-m_t))[..., None]
        b_last = b[..., -1]
        log_w = b_last[..., None] - b + ic
        m_new = jnp.maximum(b_last + m_st, jnp.max(log_w, axis=-1))
        wk = jnp.exp(log_w - m_new[..., None])
        decay = jnp.exp(b_last + m_st - m_new)
        c_new = decay[..., None, None] * c_st + jnp.einsum('bhs,bhsv,bhsd->bhvd', wk, vc, kc)
        n_new = decay[..., None] * n_st + jnp.einsum('bhs,bhsd->bhd', wk, kc)
        return (c_new, n_new, m_new), h

    state, hs = lax.scan(step, state, tuple(to_chunks(a) for a in (q, k, v, ig, lf)))
    h = jnp.moveaxis(hs, 0, 2).reshape(bsz, nh, t, -1)
    return h, state


def _mlstm_bidir(lat, ctx_in):
    ql, kl, vl, il_f, fl_f, il_b, fl_b = lat
    qc, kc, vc, ic_f, fc_f, ic_b, fc_b = ctx_in
    bsz = ql.shape[0]

    def rev(a):
        return jnp.flip(a, axis=2)

    hc_f, st_f = _mlstm_chunk_scan(qc, kc, vc, ic_f, fc_f, _mlstm_state0(bsz))
    hl_f, _ = _mlstm_chunk_scan(ql, kl, vl, il_f, fl_f, st_f)
    hc_b, st_b = _mlstm_chunk_scan(rev(qc), rev(kc), rev(vc), rev(ic_b), rev(fc_b), _mlstm_state0(bsz))
    hl_b, _ = _mlstm_chunk_scan(rev(ql), rev(kl), rev(vl), rev(il_b), rev(fl_b), st_b)
    return hl_f + rev(hl_b), hc_f + rev(hc_b)


def _mlstm_out(h, o, g, dtype):
    y = _headnorm(h.transpose(0, 2, 1, 3), g)
    return (y * jax.nn.sigmoid(o.astype(jnp.float32))).astype(dtype)


def _da_heads(q, k, v):
    bsz, t, _ = q.shape
    return (q.reshape(bsz, t, DA_HEADS, 2, DA_DQK),
            k.reshape(bsz, t, DA_HEADS, 2, DA_DQK),
            v.reshape(bsz, t, DA_HEADS, DA_DV))


def _diff_lambda(lp, lam_init):
    lp = lp.astype(jnp.float32)
    return jnp.exp(jnp.sum(lp[0] * lp[1])) - jnp.exp(jnp.sum(lp[2] * lp[3])) + lam_init


def _diff_attend(q, k, v, lam):
    s = jnp.einsum('bqhmd,bkhmd->bhmqk', q, k).astype(jnp.float32) * (DA_DQK ** -0.5)
    p = jax.nn.softmax(s, axis=-1)
    p = p[:, :, 0] - lam * p[:, :, 1]
    return jnp.einsum('bhqk,bkhv->bqhv', p.astype(v.dtype), v)


def _blocked_queries(fn, q):
    bsz, s = q.shape[:2]
    nb = s // Q_BLOCK
    qb = jnp.moveaxis(q.reshape((bsz, nb, Q_BLOCK) + q.shape[2:]), 1, 0)
    out = jnp.moveaxis(lax.map(fn, qb), 0, 1)
    return out.reshape((bsz, s) + out.shape[3:])


def _da_out(o, g, lam_init, dtype):
    return (_headnorm(o, g) * (1.0 - lam_init)).astype(dtype)


def _even_mixer(xl, xc, w_in, conv_w, conv_b, gate_b, ml_g, da_lam, da_g, w_out,
                lam_init, cos, sin, need_ctx):
    dt = xl.dtype
    pl = jnp.split(xl @ w_in, EVEN_SPLITS, axis=-1)
    pc = jnp.split(xc @ w_in, EVEN_SPLITS, axis=-1)
    lat = _mlstm_prepare(pl[0], pl[1], pl[3], conv_w, conv_b, gate_b)
    cin = _mlstm_prepare(pc[0], pc[1], pc[3], conv_w, conv_b, gate_b)
    hl, hc = _mlstm_bidir(lat, cin)
    ml_l = _mlstm_out(hl, pl[2], ml_g, dt)
    lam = _diff_lambda(da_lam, lam_init)
    ql, kl, vl = _da_heads(pl[4], pl[5], pl[6])
    qc, kc, vc = _da_heads(pc[4], pc[5], pc[6])
    ql = _apply_rope(ql, cos, sin)
    kl = _apply_rope(kl, cos, sin)
    kk = jnp.concatenate([kc, kl], axis=1)
    vv = jnp.concatenate([vc, vl], axis=1)
    da_l = _blocked_queries(lambda qb: _diff_attend(qb, kk, vv, lam), ql)
    yl = jnp.concatenate([ml_l, _da_out(da_l, da_g, lam_init, dt)], axis=-1) @ w_out
    yc = None
    if need_ctx:
        ml_c = _mlstm_out(hc, pc[2], ml_g, dt)
        da_c = _da_out(_diff_attend(qc, kc, vc, lam), da_g, lam_init, dt)
        yc = jnp.concatenate([ml_c, da_c], axis=-1) @ w_out
    return yl, yc


def _sink_softmax(s, sink):
    sk = jnp.broadcast_to(sink[None, :, :, None, None], s.shape[:-1] + (1,))
    p = jax.nn.softmax(jnp.concatenate([s, sk], axis=-1), axis=-1)
    return p[..., :-1]


def _ctx_gqa_attend(q, kc, vc, sink):
    bsz, t = q.shape[:2]
    qg = q.reshape(bsz, t, WA_KV_HEADS, WA_GROUP, WA_DH)
    s = jnp.einsum('bqhgd,bkhd->bhgqk', qg, kc).astype(jnp.float32) * (WA_DH ** -0.5)
    p = _sink_softmax(s, sink)
    o = jnp.einsum('bhgqk,bkhd->bqhgd', p.astype(vc.dtype), vc)
    return o.reshape(bsz, t, WA_W)


def _window_attend(q, k, v, kc, vc, sink):
    bsz, s = q.shape[:2]
    nb = s // W_BLOCK
    tc = kc.shape[1]
    qb = q.reshape(bsz, nb, W_BLOCK, WA_KV_HEADS, WA_GROUP, WA_DH)
    pad = ((0, 0), (W_BLOCK, W_BLOCK), (0, 0), (0, 0))
    kp = jnp.pad(k, pad).reshape(bsz, nb + 2, W_BLOCK, WA_KV_HEADS, WA_DH)
    vp = jnp.pad(v, pad).reshape(bsz, nb + 2, W_BLOCK, WA_KV_HEADS, WA_DH)
    kw = jnp.concatenate([kp[:, :-2], kp[:, 1:-1], kp[:, 2:]], axis=2)
    vw = jnp.concatenate([vp[:, :-2], vp[:, 1:-1], vp[:, 2:]], axis=2)
    qpos = jnp.arange(nb)[:, None] * W_BLOCK + jnp.arange(W_BLOCK)[None, :]
    kpos = (jnp.arange(nb)[:, None] - 1) * W_BLOCK + jnp.arange(3 * W_BLOCK)[None, :]
    mask = ((jnp.abs(qpos[:, :, None] - kpos[:, None, :]) <= WINDOW)
            & (kpos[:, None, :] >= 0) & (kpos[:, None, :] < s))
    scale = WA_DH ** -0.5

    def blk(args):
        qi, ki, vi, mi = args
        s_loc = jnp.einsum('bqhgd,bkhd->bhgqk', qi, ki).astype(jnp.float32) * scale
        s_loc = jnp.where(mi, s_loc, -jnp.inf)
        s_ctx = jnp.einsum('bqhgd,bkhd->bhgqk', qi, kc).astype(jnp.float32) * scale
        p = _sink_softmax(jnp.concatenate([s_ctx, s_loc], axis=-1), sink).astype(vi.dtype)
        return (jnp.einsum('bhgqk,bkhd->bqhgd', p[..., :tc], vc)
                + jnp.einsum('bhgqk,bkhd->bqhgd', p[..., tc:], vi))

    out = lax.map(blk, (jnp.moveaxis(qb, 1, 0), jnp.moveaxis(kw, 1, 0), jnp.moveaxis(vw, 1, 0), mask))
    return jnp.moveaxis(out, 0, 1).reshape(bsz, s, WA_W)


def _odd_mixer(xl, xc, w_in, sink, w_out, cos, sin, need_ctx):
    bsz, s, _ = xl.shape
    tc = xc.shape[1]
    ql, kl, vl = jnp.split(xl @ w_in, ODD_SPLITS, axis=-1)
    ql = _apply_rope(ql.reshape(bsz, s, WA_HEADS, WA_DH), cos, sin)
    kl = _apply_rope(kl.reshape(bsz, s, WA_KV_HEADS, WA_DH), cos, sin)
    vl = vl.reshape(bsz, s, WA_KV_HEADS, WA_DH)
    kc, vc = jnp.split(xc @ w_in[:, WA_W:], 2, axis=-1)
    kc = kc.reshape(bsz, tc, WA_KV_HEADS, WA_DH)
    vc = vc.reshape(bsz, tc, WA_KV_HEADS, WA_DH)
    sink = sink.astype(jnp.float32).reshape(WA_KV_HEADS, WA_GROUP)
    yl = _window_attend(ql, kl, vl, kc, vc, sink) @ w_out
    yc = None
    if need_ctx:
        qc = (xc @ w_in[:, :WA_W]).reshape(bsz, tc, WA_HEADS, WA_DH)
        yc = _ctx_gqa_attend(qc, kc, vc, sink) @ w_out
    return yl, yc


def _peer_ffn(x, w_q, keys, u, v):
    shp = x.shape
    xb = x.reshape(-1, PEER_TOK_BLOCK, shp[-1])

    def block(xt):
        n = xt.shape[0]
        q = (xt @ w_q).reshape(n, PEER_HEADS, 2, PEER_DHALF)
        s = jnp.einsum('nhpd,hpkd->nhpk', q, keys).astype(jnp.float32)
        sv, si = lax.top_k(s, PEER_TOPK)
        cand = sv[:, :, 0, :, None] + sv[:, :, 1, None, :]
        cidx = si[:, :, 0, :, None] * PEER_NKEYS + si[:, :, 1, None, :]
        cs, ci = lax.top_k(cand.reshape(n, PEER_HEADS, -1), PEER_TOPK)
        eidx = jnp.take_along_axis(cidx.reshape(n, PEER_HEADS, -1), ci, axis=-1)
        gate = jax.nn.softmax(cs, axis=-1)
        a = jnp.einsum('nd,nhkd->nhk', xt, u[eidx]).astype(jnp.float32)
        w = (gate * jax.nn.gelu(a, approximate=False)).astype(xt.dtype)
        return jnp.einsum('nhk,nhkd->nd', w, v[eidx])

    return lax.map(block, xb).reshape(shp)


def setup_inputs(seed: int = 0) -> dict:
    key = jax.random.key(seed)
    ks = iter(jax.random.split(key, 32))

    def nrm(shape, scale):
        return jax.random.normal(next(ks), shape, jnp.float32) * scale

    d = D_MODEL
    zeros_h = jnp.zeros((ML_HEADS,), jnp.float32)
    f_bias = jnp.linspace(3.0, 6.0, ML_HEADS, dtype=jnp.float32)
    gate_base = jnp.concatenate([zeros_h, f_bias, zeros_h, f_bias])
    return {
        'x': nrm((BATCH, SEQ, d), 1.0),
        'c': nrm((BATCH, d), 1.0),
        'ctx': nrm((BATCH, CTX_LEN, d), 1.0),
        'c_ctx': nrm((d,), 1.0),
        'mod_w': nrm((DEPTH, d, N_MOD * d), 0.5 * d ** -0.5),
        'mod_b': nrm((DEPTH, N_MOD * d), 0.02),
        'norm1_g': 1.0 + nrm((DEPTH, d), 0.02),
        'norm2_g': 1.0 + nrm((DEPTH, d), 0.02),
        'ev_w_in': nrm((N_EVEN, d, EVEN_IN), d ** -0.5),
        'ev_ml_conv_w': nrm((N_EVEN, ML_CONV, 2 * ML_HEADS * ML_DQK), ML_CONV ** -0.5),
        'ev_ml_conv_b': nrm((N_EVEN, 2 * ML_HEADS * ML_DQK), 0.02),
        'ev_ml_gate_b': gate_base[None, :] + nrm((N_EVEN, 4 * ML_HEADS), 0.1),
        'ev_ml_norm_g': 1.0 + nrm((N_EVEN, ML_W), 0.02),
        'ev_da_lam': nrm((N_EVEN, 4, DA_DQK), 0.1),
        'ev_da_norm_g': 1.0 + nrm((N_EVEN, DA_W), 0.02),
        'ev_w_out': nrm((N_EVEN, MIX_W, d), MIX_W ** -0.5),
        'od_w_in': nrm((N_ODD, d, ODD_IN), d ** -0.5),
        'od_sink': nrm((N_ODD, WA_HEADS), 1.0),
        'od_w_out': nrm((N_ODD, WA_W, d), WA_W ** -0.5),
        'pr_w_q': nrm((DEPTH, d, PEER_HEADS * PEER_DQ), d ** -0.5),
        'pr_keys': nrm((DEPTH, PEER_HEADS, 2, PEER_NKEYS, PEER_DHALF), PEER_DHALF ** -0.5),
        'pr_u': nrm((DEPTH, PEER_EXPERTS, d), d ** -0.5),
        'pr_v': nrm((DEPTH, PEER_EXPERTS, d), PEER_HEADS ** -0.5),
        'final_g': 1.0 + nrm((d,), 0.02),
    }


def reference(x, c, ctx, c_ctx, mod_w, mod_b, norm1_g, norm2_g, ev_w_in, ev_ml_conv_w,
              ev_ml_conv_b, ev_ml_gate_b, ev_ml_norm_g, ev_da_lam, ev_da_norm_g, ev_w_out,
              od_w_in, od_sink, od_w_out, pr_w_q, pr_keys, pr_u, pr_v, final_g):
    bsz, s, d = x.shape
    cos_da, sin_da = _axial_rope_tables(s, DA_DQK)
    cos_wa, sin_wa = _axial_rope_tables(s, WA_DH)
    sc = jax.nn.silu(c)
    scc = jax.nn.silu(c_ctx)
    hl, hc = x, ctx
    for i in range(DEPTH):
        need_ctx = i < DEPTH - 1
        j = i // 2
        ml = (sc @ mod_w[i] + mod_b[i]).reshape(bsz, 1, N_MOD, d)
        mc = (scc @ mod_w[i] + mod_b[i]).reshape(N_MOD, d)
        xl = _rmsnorm(hl, norm1_g[i]) * (1.0 + ml[:, :, 1]) + ml[:, :, 0]
        xc = _rmsnorm(hc, norm1_g[i]) * (1.0 + mc[1]) + mc[0]
        if i % 2 == 0:
            lam_init = 0.8 - 0.6 * math.exp(-0.3 * i)
            yl, yc = _even_mixer(xl, xc, ev_w_in[j], ev_ml_conv_w[j], ev_ml_conv_b[j],
                                 ev_ml_gate_b[j], ev_ml_norm_g[j], ev_da_lam[j], ev_da_norm_g[j],
                                 ev_w_out[j], lam_init, cos_da, sin_da, need_ctx)
        else:
            yl, yc = _odd_mixer(xl, xc, od_w_in[j], od_sink[j], od_w_out[j],
                                cos_wa, sin_wa, need_ctx)
        hl = hl + ml[:, :, 2] * yl
        xl = _rmsnorm(hl, norm2_g[i]) * (1.0 + ml[:, :, 4]) + ml[:, :, 3]
        hl = hl + ml[:, :, 5] * _peer_ffn(xl, pr_w_q[i], pr_keys[i], pr_u[i], pr_v[i])
        if need_ctx:
            hc = hc + mc[2] * yc
            xc = _rmsnorm(hc, norm2_g[i]) * (1.0 + mc[4]) + mc[3]
            hc = hc + mc[5] * _peer_ffn(xc, pr_w_q[i], pr_keys[i], pr_u[i], pr_v[i])
    return _rmsnorm(hl, final_g)
```

```python
from contextlib import ExitStack
import numpy as np
import concourse.bass as bass
import concourse.mybir as mybir
from concourse.bass_utils import run_bass_kernel_spmd

F32 = mybir.dt.float32
BF16 = mybir.dt.bfloat16
ALU = mybir.AluOpType
AF = mybir.ActivationFunctionType
AX = mybir.AxisListType

N_DMA_SEMS = 24


class _Op:
    __slots__ = ("eng", "fn", "reads", "writes", "waits", "signal", "sem", "val", "idx", "kind")

    def __init__(self, eng, fn, reads, writes, kind="op"):
        self.eng = eng
        self.fn = fn
        self.reads = tuple(reads)
        self.writes = tuple(writes)
        self.waits = []
        self.signal = False
        self.sem = None
        self.val = 0
        self.kind = kind


class Prog:
    ENGS = ("pe", "act", "dve", "pool", "sp")

    def __init__(self, nc):
        self.nc = nc
        self.ops = []
        self.stack = ExitStack()
        self._n = 0

    def sb(self, name, shape, dtype):
        return self.stack.enter_context(self.nc.sbuf_tensor(name, list(shape), dtype))

    def ps(self, name, shape, dtype=F32):
        return self.stack.enter_context(self.nc.psum_tensor(name, list(shape), dtype))

    def _add(self, eng, fn, reads, writes, kind="op"):
        op = _Op(eng, fn, reads, writes, kind)
        op.idx = len(self.ops)
        self.ops.append(op)
        return op

    def pe(self, fn, reads=(), writes=()):
        return self._add("pe", fn, reads, writes)

    def act(self, fn, reads=(), writes=()):
        return self._add("act", fn, reads, writes)

    def dve(self, fn, reads=(), writes=()):
        return self._add("dve", fn, reads, writes)

    def pool(self, fn, reads=(), writes=()):
        return self._add("pool", fn, reads, writes)

    def barrier(self):
        for e in self.ENGS:
            self._add(e, None, [], [], kind="barrier")

    def dma(self, out, in_, reads=(), writes=(), eng="sp", **kw):
        return self._add(eng, lambda e: e.dma_start(out=out, in_=in_, **kw), reads, writes, kind="dma")

    def finish(self, outputs=()):
        nc = self.nc
        ops = self.ops
        fence = self._add("sp", None, list(outputs), [], kind="fence")
        last_w = {}
        readers = {}
        deps_of = []
        last_on_eng = {}
        all_dmas = []
        for op in ops:
            deps = {}
            if op.kind == "barrier":
                for p in last_on_eng.values():
                    deps[p] = True
                for p in all_dmas:
                    deps[p] = True
                deps_of.append(deps)
                continue
            if op.kind == "dma":
                all_dmas.append(op.idx)
            elif op.kind == "op":
                last_on_eng[op.eng] = op.idx
            for r in op.reads:
                p = last_w.get(r)
                if p is not None:
                    deps[p] = True
            for w in op.writes:
                p = last_w.get(w)
                if p is not None and p not in deps:
                    deps[p] = False
                for rd in readers.get(w, ()):
                    if rd not in deps:
                        deps[rd] = False
            deps.pop(op.idx, None)
            for r in op.reads:
                readers.setdefault(r, []).append(op.idx)
            for w in op.writes:
                last_w[w] = op.idx
                readers[w] = []
            deps_of.append(deps)
        for op, deps in zip(ops, deps_of):
            keep = []
            for p, raw in deps.items():
                pop = ops[p]
                if pop.kind == "dma":
                    keep.append(p)
                elif pop.eng == op.eng:
                    if op.eng != "pe":
                        keep.append(p)
                else:
                    keep.append(p)
            op.waits = sorted(keep)
            for p in keep:
                ops[p].signal = True
        sems = {e: self.stack.enter_context(nc.semaphore("s_" + e)) for e in ("pe", "act", "dve", "pool")}
        dsems = [self.stack.enter_context(nc.semaphore("d%d" % i)) for i in range(N_DMA_SEMS)]
        cnt = {e: 0 for e in sems}
        ndma = 0
        dma_prev = {}
        for op in ops:
            if op.kind == "dma":
                k = ndma % N_DMA_SEMS
                op.sem = dsems[k]
                op.val = 16 * (ndma // N_DMA_SEMS + 1)
                prev = dma_prev.get(k)
                if prev is not None and prev.idx not in op.waits:
                    op.waits.append(prev.idx)
                dma_prev[k] = op
                op.signal = True
                ndma += 1
            elif op.kind == "op" and op.signal:
                cnt[op.eng] += 1
                op.sem = sems[op.eng]
                op.val = cnt[op.eng]
        per_eng = {e: [op for op in ops if op.eng == e] for e in self.ENGS}
        self.stats = {e: len(v) for e, v in per_eng.items()}

        def emit(engname, eng):
            waited = {}
            for op in per_eng[engname]:
                need = {}
                for p in op.waits:
                    pop = ops[p]
                    key = id(pop.sem)
                    if waited.get(key, 0) >= pop.val:
                        continue
                    cur = need.get(key)
                    if cur is None or cur[1] < pop.val:
                        need[key] = (pop.sem, pop.val)
                for key, (s, v) in need.items():
                    eng.wait_ge(s, v)
                    waited[key] = v
                if op.kind in ("fence", "barrier"):
                    continue
                inst = op.fn(eng)
                if op.signal:
                    inst.then_inc(op.sem, 16 if op.kind == "dma" else 1)

        with nc.Block() as block:
            @block.sync
            def _(e):
                emit("sp", e)

            @block.tensor
            def _(e):
                emit("pe", e)

            @block.scalar
            def _(e):
                emit("act", e)

            @block.vector
            def _(e):
                emit("dve", e)

            @block.gpsimd
            def _(e):
                emit("pool", e)
        self.stack.close()


def _tt(P, eng, out, in0, in1, op, r, w):
    return P._add(eng, lambda e: e.tensor_tensor(out=out, in0=in0, in1=in1, op=op), r, w)


def _ts(P, eng, out, in0, s1, s2, op0, op1, r, w, accum_out=None):
    if accum_out is not None:
        return P._add(eng, lambda e: e.tensor_scalar(out=out, in0=in0, scalar1=s1, scalar2=s2, op0=op0, op1=op1,
                                                     accum_out=accum_out), r, w)
    if op1 is None:
        return P._add(eng, lambda e: e.tensor_scalar(out=out, in0=in0, scalar1=s1, scalar2=None, op0=op0), r, w)
    return P._add(eng, lambda e: e.tensor_scalar(out=out, in0=in0, scalar1=s1, scalar2=s2, op0=op0, op1=op1), r, w)


def _stt(P, eng, out, in0, scalar, in1, op0, op1, r, w):
    return P._add(eng, lambda e: e.scalar_tensor_tensor(out=out, in0=in0, scalar=scalar, in1=in1, op0=op0, op1=op1), r, w)


def _act(P, out, in_, func, r, w, bias=None, scale=None, accum_out=None):
    kw = {}
    if bias is not None:
        kw["bias"] = bias
    if scale is not None:
        kw["scale"] = scale
    if accum_out is not None:
        kw["accum_out"] = accum_out
    return P._add("act", lambda e: e.activation(out=out, in_=in_, func=func, **kw), r, w)


def _cp(P, eng, out, in_, r, w):
    if eng == "act":
        return P._add("act", lambda e: e.activation(out=out, in_=in_, func=AF.Copy), r, w)
    return P._add(eng, lambda e: e.tensor_copy(out=out, in_=in_), r, w)


def _mm(P, out, lhsT, rhs, start, stop, r, w):
    return P._add("pe", lambda e: e.matmul(out, lhsT, rhs, start=start, stop=stop), r, w)


def _tr(P, out, in_, ident, r, w):
    return P._add("pe", lambda e: e.transpose(out=out, in_=in_, identity=ident), r, w)


def _memset(P, eng, ap, val, w):
    return P._add(eng, lambda e: e.memset(ap, val), [], w)


class Arena:
    def __init__(self, P, name, nbytes):
        self.words = nbytes // 4
        self.t = P.sb(name, [128, self.words], F32)
        self.off = 0

    def reset(self):
        self.off = 0

    def alloc(self, shape, dtype, parts=128):
        n = 1
        for s in shape:
            n *= s
        words = n if dtype == F32 else (n + 1) // 2
        words = (words + 7) // 8 * 8
        assert self.off + words <= self.words, ("arena overflow", self.off, words, self.words)
        ap = self.t[0:parts, self.off:self.off + words]
        self.off += words
        if dtype != F32:
            ap = ap.bitcast(dtype)
        ap = ap[:, 0:n]
        if len(shape) == 2:
            ap = ap.rearrange("p (a b) -> p a b", a=shape[0])
        elif len(shape) == 3:
            ap = ap.rearrange("p (a b c) -> p a b c", a=shape[0], b=shape[1])
        elif len(shape) == 4:
            ap = ap.rearrange("p (a b c d) -> p a b c d", a=shape[0], b=shape[1], c=shape[2])
        return ap


D = 1024
KC = 8
NEXP = 16384
EVEN_IN = 3088
ODD_IN = 1536
EPS = 1e-6
GRP = 6
ECH = 512


def build_program(NLB, NCB, DEPTH, only=None):
    nc = bass.Bass("TRN2", target_bir_lowering=False)
    NB = NLB + NCB
    T = NB * 128
    TL = NLB * 128
    NE = (DEPTH + 1) // 2
    NO = max(DEPTH // 2, 1)

    def dram(name, shape, kind="ExternalInput"):
        return nc.dram_tensor(name, list(shape), F32, kind=kind).ap()

    x_d = dram("x", [TL, D])
    ctx_d = dram("ctx", [NCB * 128, D])
    c2_d = dram("c2", [2, D])
    mod_w_d = dram("mod_w", [DEPTH, D, 6 * D])
    mod_b_d = dram("mod_b", [DEPTH, 6 * D])
    n1g_d = dram("norm1_g", [DEPTH, D])
    n2g_d = dram("norm2_g", [DEPTH, D])
    ev_w_in_d = dram("ev_w_in", [NE, D, EVEN_IN])
    ev_w_sw_d = dram("ev_w_sw", [NE, D, 1024])
    conv_w_d = dram("ev_ml_conv_w", [NE, 3, 512])
    conv_b_d = dram("ev_ml_conv_b", [NE, 512])
    gate_b_d = dram("ev_ml_gate_b", [NE, 16])
    ml_g_d = dram("ev_ml_norm_g", [NE, 512])
    da_lam_d = dram("ev_da_lam", [NE, 256])
    da_g_d = dram("ev_da_norm_g", [NE, 512])
    ev_w_out_d = dram("ev_w_out", [NE, D, D])
    od_w_in_d = dram("od_w_in", [NO, D, ODD_IN])
    od_w_sw_d = dram("od_w_sw", [NO, D, 1280])
    od_sink_d = dram("od_sink", [NO, 16])
    od_w_out_d = dram("od_w_out", [NO, D, D])
    pr_w_q_d = dram("pr_w_q", [DEPTH, D, 2048])
    pr_keysT_d = dram("pr_keysT", [DEPTH, 16, 128, 128])
    pr_uT_d = dram("pr_uT", [DEPTH, D, NEXP])
    pr_v_d = dram("pr_v", [DEPTH, NEXP, D])
    final_g_d = dram("final_g", [1, D])
    rope_cos_d = dram("rope_cos", [128, TL])
    rope_sin_d = dram("rope_sin", [128, TL])
    out_d = dram("out", [TL, D], kind="ExternalOutput")
    hbuf_d = dram("hbuf", [T, D], kind="Internal")

    P = Prog(nc)
    identb = P.sb("identb", [128, 128], BF16)
    identf = P.sb("identf", [128, 128], F32)
    tri_le = P.sb("tri_le", [128, 128], BF16)
    tri_ge = P.sb("tri_ge", [128, 128], BF16)
    sel = P.sb("sel", [2, 2, 128], F32)
    xT = P.sb("xT", [128, KC, T], BF16)
    hA = P.sb("hA", [128, D], F32)
    hB = P.sb("hB", [128, D], F32)
    hC = P.sb("hC", [128, D], BF16)
    gates = P.sb("gates", [128, 4, D], F32)
    cols = P.sb("cols", [128, 4, KC, 2], F32)
    small = P.sb("small", [128, 64], F32)
    ps = [P.ps("ps%d" % i, [128, 512], F32) for i in range(8)]
    psb = [p[:].bitcast(BF16) for p in ps]
    AR = Arena(P, "arena", 135 * 1024)
    stg = P.sb("stg", [128, 3, 512], F32)
    stg_n = [0]

    def load_cast(dst, src, wname):
        A_, B_ = dst.shape[1], dst.shape[2]
        if B_ >= 512:
            pieces = [(a, 1, b0, min(512, B_ - b0)) for a in range(A_) for b0 in range(0, B_, 512)]
        else:
            per = max(1, 512 // B_)
            pieces = [(a, min(per, A_ - a), 0, B_) for a in range(0, A_, per)]
        for (a0, an, b0, bn) in pieces:
            k = stg_n[0] % 3
            stg_n[0] += 1
            sv = stg[:, k, 0:an * bn].rearrange("p (a b) -> p a b", a=an)
            P.dma(sv, src[:, a0:a0 + an, b0:b0 + bn], reads=[], writes=[("stg", k)])
            _cp(P, "act", dst[:, a0:a0 + an, b0:b0 + bn], sv, [("stg", k)], [wname])

    def PSr(i):
        return ("ps", i)

    XTALL = [("xT", b_) for b_ in range(NB)]

    _memset(P, "pool", identb[:], 0.0, ["identb"])
    P._add("pool", lambda e: e.affine_select(out=identb[:], in_=identb[:], pattern=[[-1, 128]], compare_op=ALU.not_equal,
                                             fill=1.0, base=0, channel_multiplier=1), ["identb"], ["identb"])
    _memset(P, "pool", identf[:], 0.0, ["identf"])
    P._add("pool", lambda e: e.affine_select(out=identf[:], in_=identf[:], pattern=[[-1, 128]], compare_op=ALU.not_equal,
                                             fill=1.0, base=0, channel_multiplier=1), ["identf"], ["identf"])
    _memset(P, "pool", tri_le[:], 1.0, ["tri_le"])
    P._add("pool", lambda e: e.affine_select(out=tri_le[:], in_=tri_le[:], pattern=[[1, 128]], compare_op=ALU.is_ge,
                                             fill=0.0, base=0, channel_multiplier=-1), ["tri_le"], ["tri_le"])
    _memset(P, "pool", tri_ge[:], 1.0, ["tri_ge"])
    P._add("pool", lambda e: e.affine_select(out=tri_ge[:], in_=tri_ge[:], pattern=[[-1, 128]], compare_op=ALU.is_ge,
                                             fill=0.0, base=0, channel_multiplier=1), ["tri_ge"], ["tri_ge"])
    _memset(P, "pool", sel[:], 1.0, ["sel"])
    P._add("pool", lambda e: e.affine_select(out=sel[:, 0, :], in_=sel[:, 0, :], pattern=[[0, 128]], compare_op=ALU.is_equal,
                                             fill=0.0, base=0, channel_multiplier=1), ["sel"], ["sel"])
    P._add("pool", lambda e: e.affine_select(out=sel[:, 1, :], in_=sel[:, 1, :], pattern=[[0, 128]], compare_op=ALU.is_equal,
                                             fill=0.0, base=-1, channel_multiplier=1), ["sel"], ["sel"])

    hver = [0] * NB

    def h_src(blk):
        if hver[blk] == 0:
            if blk < NLB:
                return x_d[blk * 128:(blk + 1) * 128, :]
            return ctx_d[(blk - NLB) * 128:(blk - NLB + 1) * 128, :]
        return hbuf_d[blk * 128:(blk + 1) * 128, :]

    def load_h(blk, tile, tname):
        P.dma(tile[:], h_src(blk), reads=[("h", blk)], writes=[tname])

    def store_h(blk, tile, tname):
        P.dma(hbuf_d[blk * 128:(blk + 1) * 128, :], tile[:], reads=[tname], writes=[("h", blk)])
        hver[blk] = 1

    def norm_to_xT(blk, tile, tname, which):
        lc = 0 if blk < NLB else 1
        ss = small[:, 0:1]
        rs = small[:, 1:2]
        _act(P, hC[:], tile[:], AF.Square, [tname], ["hC", "ss"], accum_out=ss)
        _ts(P, "dve", rs, ss, 1.0 / D, EPS, ALU.mult, ALU.add, ["ss"], ["rs"])
        _act(P, rs, rs, AF.Sqrt, ["rs"], ["rs"])
        P._add("dve", lambda e: e.reciprocal(out=rs, in_=rs), ["rs"], ["rs"])
        _ts(P, "dve", hC[:], tile[:], rs, None, ALU.mult, None, [tname, "rs", "hC"], ["hC"])
        for kc in range(KC):
            _tr(P, psb[7][:, kc * 128:(kc + 1) * 128], hC[:, kc * 128:(kc + 1) * 128], identb[:], ["hC", "identb"], [PSr(7)])
        for kc in range(KC):
            _ts(P, "dve", xT[:, kc, blk * 128:(blk + 1) * 128], psb[7][:, kc * 128:(kc + 1) * 128],
                cols[:, 2 * which, kc, lc:lc + 1], cols[:, 2 * which + 1, kc, lc:lc + 1], ALU.mult, ALU.add,
                [PSr(7), "cols"], [("xT", blk)])

    def modvec(l):
        AR.reset()
        cT = AR.alloc([KC, 2], F32)
        rows = AR.alloc([6 * D], F32, parts=2)
        wst = AR.alloc([2, KC, 512], F32)
        gcol = AR.alloc([2, KC], F32)
        for r_ in range(2):
            P.dma(cT[:, :, r_], c2_d[r_:r_ + 1, :].rearrange("o (k p) -> p (o k)", p=128), reads=[], writes=["cT"],
                  allow_slow_non_contiguous=True)
        P.dma(gcol[:, 0, :], n1g_d[l:l + 1, :].rearrange("o (k p) -> p (o k)", p=128), reads=[], writes=["gcol"],
              allow_slow_non_contiguous=True)
        P.dma(gcol[:, 1, :], n2g_d[l:l + 1, :].rearrange("o (k p) -> p (o k)", p=128), reads=[], writes=["gcol"],
              allow_slow_non_contiguous=True)
        _act(P, cT, cT, AF.Silu, ["cT"], ["cT"])
        P.dma(rows[0:1, :], mod_b_d[l:l + 1, :], reads=[], writes=["rows"])
        P.dma(rows[1:2, :], mod_b_d[l:l + 1, :], reads=[], writes=["rows"])
        for n in range(12):
            b = n % 2
            P.dma(wst[:, b], mod_w_d[l].rearrange("(k p) n -> p k n", p=128)[:, :, n * 512:(n + 1) * 512],
                  reads=[], writes=[("wst", b)])
            for kc in range(KC):
                _mm(P, ps[0][0:2, :], cT[:, kc, :], wst[:, b, kc, :], kc == 0, kc == KC - 1,
                    ["cT", ("wst", b)], [PSr(0)])
            _tt(P, "dve", rows[0:2, n * 512:(n + 1) * 512], ps[0][0:2, :], rows[0:2, n * 512:(n + 1) * 512], ALU.add,
                [PSr(0), "rows"], ["rows"])
        for gi, idx in enumerate((2, 5)):
            for lc in range(2):
                for half in range(2):
                    _mm(P, ps[1][:, :], sel[:, lc, :], rows[0:2, idx * D + half * 512: idx * D + (half + 1) * 512],
                        True, True, ["sel", "rows"], [PSr(1)])
                    _cp(P, "act", gates[:, gi * 2 + lc, half * 512:(half + 1) * 512], ps[1][:, :], [PSr(1)], ["gates"])
        for wi, (sh, sc) in enumerate(((0, 1), (3, 4))):
            for kc in range(KC):
                _tr(P, ps[2][:, kc * 4:kc * 4 + 2], rows[0:2, sc * D + kc * 128: sc * D + (kc + 1) * 128], identf[0:2, 0:2],
                    ["rows", "identf"], [PSr(2)])
                _tr(P, ps[2][:, kc * 4 + 2:kc * 4 + 4], rows[0:2, sh * D + kc * 128: sh * D + (kc + 1) * 128], identf[0:2, 0:2],
                    ["rows", "identf"], [PSr(2)])
            for kc in range(KC):
                for lc in range(2):
                    _stt(P, "dve", cols[:, 2 * wi, kc, lc:lc + 1], ps[2][:, kc * 4 + lc:kc * 4 + lc + 1], 1.0,
                         gcol[:, wi, kc:kc + 1], ALU.add, ALU.mult, [PSr(2), "gcol"], ["cols"])
                _cp(P, "dve", cols[:, 2 * wi + 1, kc, :], ps[2][:, kc * 4 + 2:kc * 4 + 4], [PSr(2)], ["cols"])
        P.barrier()

    def peer(l, blocks, after_block):
        AR.reset()
        keysT = AR.alloc([16, 128], BF16)
        load_cast(keysT, pr_keysT_d[l].rearrange("f d k -> d f k"), "keysT")
        wq = AR.alloc([2, KC, 128], BF16)
        qTs = AR.alloc([GRP * 128], BF16)
        sst = AR.alloc([GRP, 16, 128], F32)
        acc = AR.alloc([GRP, D], F32)
        uT = AR.alloc([2, KC, ECH], BF16)
        vch = AR.alloc([2, ECH // 128, D], BF16)
        v16 = AR.alloc([16, 16], F32)
        tmp128 = AR.alloc([128], F32)
        cand = AR.alloc([256], F32)
        cand2 = AR.alloc([256], F32)
        cand3 = AR.alloc([256], F32)
        ctop = AR.alloc([8, 24], F32)
        ez = AR.alloc([8, 16], F32)
        st = AR.alloc([GRP, 8, 4], F32)
        nb1 = AR.alloc([8], F32)
        nb2 = AR.alloc([8], F32)
        zz = AR.alloc([8], F32)
        gA = AR.alloc([ECH], F32)
        ex = AR.alloc([2, ECH], F32)
        gg = AR.alloc([2, ECH], F32)
        gacc = AR.alloc([ECH], F32)
        Wb = AR.alloc([ECH], BF16)
        WT = AR.alloc([ECH // 128, 128], BF16)
        NCH = NEXP // ECH
        IPC = ECH // 128
        groups = [blocks[i:i + GRP] for i in range(0, len(blocks), GRP)]
        for grp in groups:
            ng = len(grp)
            for fc in range(16):
                b = fc % 2
                load_cast(wq[:, b], pr_w_q_d[l].rearrange("(k p) n -> p k n", p=128)[:, :, fc * 128:(fc + 1) * 128], ("wq", b))
                for gi, blk in enumerate(grp):
                    for kc in range(KC):
                        _mm(P, ps[0][:, gi * 128 % 512:gi * 128 % 512 + 128] if gi < 4 else ps[1][:, (gi - 4) * 128:(gi - 3) * 128],
                            wq[:, b, kc, :], xT[:, kc, blk * 128:(blk + 1) * 128], kc == 0, kc == KC - 1,
                            [("wq", b), ("xT", blk)], [PSr(0 if gi < 4 else 1)])
                n0 = min(ng, 4)
                _cp(P, "act", qTs[:, 0:n0 * 128], ps[0][:, 0:n0 * 128], [PSr(0)], ["qTs"])
                if ng > 4:
                    _cp(P, "act", qTs[:, 512:ng * 128], ps[1][:, 0:(ng - 4) * 128], [PSr(1)], ["qTs"])
                for gi, blk in enumerate(grp):
                    pb = 2 + (gi // 4)
                    _mm(P, ps[pb][:, (gi % 4) * 128:(gi % 4 + 1) * 128], qTs[:, gi * 128:(gi + 1) * 128], keysT[:, fc, :],
                        True, True, ["qTs", "keysT"], [PSr(pb)])
                _cp(P, "dve", sst[:, 0:n0, fc, :], ps[2][:, 0:n0 * 128].rearrange("p (g k) -> p g k", g=n0), [PSr(2)], ["sst"])
                if ng > 4:
                    _cp(P, "dve", sst[:, 4:ng, fc, :], ps[3][:, 0:(ng - 4) * 128].rearrange("p (g k) -> p g k", g=ng - 4),
                        [PSr(3)], ["sst"])
            for gi, blk in enumerate(grp):
                for fc in range(16):
                    P._add("dve", lambda e, fc=fc, gi=gi: e.max(out=v16[:, fc, 0:8], in_=sst[:, gi, fc, :]), ["sst"], ["v16"])
                    P._add("dve", lambda e, fc=fc, gi=gi: e.match_replace(out=tmp128, in_to_replace=v16[:, fc, 0:8],
                                                                         in_values=sst[:, gi, fc, :], imm_value=-1e30),
                           ["sst", "v16"], ["tmp128"])
                    P._add("dve", lambda e, fc=fc: e.max(out=v16[:, fc, 8:16], in_=tmp128), ["tmp128"], ["v16"])
                v4 = v16.rearrange("p (h t) k -> p h t k", t=2)
                for h in range(8):
                    _tt(P, "dve", cand.rearrange("p (a b) -> p a b", a=16),
                        v4[:, h, 0, :].unsqueeze(2).to_broadcast([128, 16, 16]),
                        v4[:, h, 1, :].unsqueeze(1).to_broadcast([128, 16, 16]), ALU.add, ["v16"], ["cand"])
                    P._add("dve", lambda e, h=h: e.max(out=ctop[:, h, 0:8], in_=cand), ["cand"], ["ctop"])
                    P._add("dve", lambda e, h=h: e.match_replace(out=cand2, in_to_replace=ctop[:, h, 0:8], in_values=cand,
                                                                 imm_value=-1e30), ["cand", "ctop"], ["cand2"])
                    P._add("dve", lambda e, h=h: e.max(out=ctop[:, h, 8:16], in_=cand2), ["cand2"], ["ctop"])
                    P._add("dve", lambda e, h=h: e.match_replace(out=cand3, in_to_replace=ctop[:, h, 8:16], in_values=cand2,
                                                                 imm_value=-1e30), ["cand2", "ctop"], ["cand3"])
                    P._add("dve", lambda e, h=h: e.max(out=ctop[:, h, 16:24], in_=cand3), ["cand3"], ["ctop"])
                _tt(P, "dve", ez, ctop[:, :, 0:16], ctop[:, :, 0:1].to_broadcast([128, 8, 16]), ALU.subtract, ["ctop"], ["ez"])
                _act(P, ez, ez, AF.Exp, ["ez"], ["ez"])
                P._add("dve", lambda e: e.tensor_reduce(out=zz, in_=ez, axis=AX.X, op=ALU.add), ["ez"], ["zz"])
                _act(P, zz, zz, AF.Ln, ["zz"], ["zz"])
                _ts(P, "dve", nb1, v4[:, :, 0, 0], -1.0, None, ALU.mult, None, ["v16"], ["nb1"])
                _stt(P, "dve", nb2, v4[:, :, 1, 0], -1.0, zz, ALU.mult, ALU.subtract, ["v16", "zz"], ["nb2"])
                thr = st[:, gi, :, 1]
                _tt(P, "dve", thr, ctop[:, :, 15], ctop[:, :, 16], ALU.add, ["ctop"], ["st"])
                _stt(P, "dve", thr, thr, 0.5, ctop[:, :, 0], ALU.mult, ALU.subtract, ["st", "ctop"], ["st"])
                _tt(P, "dve", thr, thr, zz, ALU.subtract, ["st", "zz"], ["st"])
                _act(P, st[:, gi, :, 0], thr, AF.Exp, ["st"], ["st"])
                for h in range(8):
                    _act(P, sst[:, gi, 2 * h, :], sst[:, gi, 2 * h, :], AF.Exp, ["sst", "nb1"], ["sst"], bias=nb1[:, h:h + 1])
                    _act(P, sst[:, gi, 2 * h + 1, :], sst[:, gi, 2 * h + 1, :], AF.Exp, ["sst", "nb2"], ["sst"],
                         bias=nb2[:, h:h + 1])
            def load_chunk(c):
                b = c % 2
                load_cast(uT[:, b], pr_uT_d[l].rearrange("(k p) e -> p k e", p=128)[:, :, c * ECH:(c + 1) * ECH], ("uT", b))
                load_cast(vch[:, b], pr_v_d[l][c * ECH:(c + 1) * ECH, :].rearrange("(s p) d -> p s d", p=128), ("vch", b))
            load_chunk(0)
            for c in range(NCH):
                b = c % 2
                if c + 1 < NCH:
                    load_chunk(c + 1)
                for gi, blk in enumerate(grp):
                    pa = 0 + (gi % 2)
                    for kc in range(KC):
                        _mm(P, ps[pa][:, :], xT[:, kc, blk * 128:(blk + 1) * 128], uT[:, b, kc, :], kc == 0, kc == KC - 1,
                            [("xT", blk), ("uT", b)], [PSr(pa)])
                    _act(P, gA, ps[pa][:, :], AF.Gelu, [PSr(pa)], ["gA"])
                    for h in range(8):
                        eb = h % 2
                        e1 = sst[:, gi, 2 * h, c * IPC:(c + 1) * IPC]
                        e2 = sst[:, gi, 2 * h + 1, :]
                        _tt(P, "pool", ex[:, eb].rearrange("p (i j) -> p i j", i=IPC),
                            e1.unsqueeze(2).to_broadcast([128, IPC, 128]), e2.unsqueeze(1).to_broadcast([128, IPC, 128]),
                            ALU.mult, ["sst"], [("ex", eb)])
                        dst = gacc if h == 0 else gg[:, eb]
                        dname = "gacc" if h == 0 else ("gg", eb)
                        _stt(P, "dve", dst, ex[:, eb], st[:, gi, h, 0:1], ex[:, eb], ALU.is_ge, ALU.mult,
                             [("ex", eb), "st"], [dname])
                        if h > 0:
                            _tt(P, "dve", gacc, gacc, gg[:, eb], ALU.add, ["gacc", ("gg", eb)], ["gacc"])
                    _tt(P, "dve", Wb, gacc, gA, ALU.mult, ["gacc", "gA"], ["Wb"])
                    for s_ in range(ECH // 128):
                        _tr(P, psb[2][:, s_ * 128:(s_ + 1) * 128], Wb[:, s_ * 128:(s_ + 1) * 128], identb[:],
                            ["Wb", "identb"], [PSr(2)])
                    _cp(P, "act", WT, psb[2][:, 0:ECH].rearrange("p (s t) -> p s t", s=ECH // 128), [PSr(2)], ["WT"])
                    for half in range(2):
                        for s_ in range(ECH // 128):
                            _mm(P, ps[4 + half][:, :], WT[:, s_, :], vch[:, b, s_, half * 512:(half + 1) * 512],
                                s_ == 0, s_ == ECH // 128 - 1, ["WT", ("vch", b)], [PSr(4 + half)])
                        if c == 0:
                            _cp(P, "dve", acc[:, gi, half * 512:(half + 1) * 512], ps[4 + half][:, :], [PSr(4 + half)],
                                [("acc", gi)])
                        else:
                            _tt(P, "dve", acc[:, gi, half * 512:(half + 1) * 512], ps[4 + half][:, :],
                                acc[:, gi, half * 512:(half + 1) * 512], ALU.add, [PSr(4 + half), ("acc", gi)], [("acc", gi)])
            for gi, blk in enumerate(grp):
                lc = 0 if blk < NLB else 1
                load_h(blk, hA, "hA")
                _tt(P, "dve", acc[:, gi, :], acc[:, gi, :], gates[:, 2 + lc, :], ALU.mult, [("acc", gi), "gates"], [("acc", gi)])
                _tt(P, "dve", hA[:], hA[:], acc[:, gi, :], ALU.add, ["hA", ("acc", gi)], ["hA"])
                after_block(blk, hA, "hA")
        P.barrier()

    def attend_step(qT_ap, kT_ap, vaug_ap, out_ps, out_res, first, last, func, scale, mask, r_q, r_k, r_v, sbank, Et, Ename):
        _mm(P, ps[sbank][:, 0:128], kT_ap, qT_ap, True, True, [r_q, r_k], [PSr(sbank)])
        _act(P, Et, ps[sbank][:, 0:128], func, [PSr(sbank)] + ([scale[1]] if isinstance(scale, tuple) else []), [Ename],
             scale=(scale[0] if isinstance(scale, tuple) else scale))
        if mask is not None:
            _tt(P, "pool", Et, Et, mask[:], ALU.mult, [Ename], [Ename])
        _mm(P, out_ps, Et, vaug_ap, first, last, [Ename, r_v], [out_res])

    def odd_mixer(l, j, need_ctx, after_block):
        AR.reset()
        cosT = AR.alloc([TL], F32)
        sinT = AR.alloc([TL], F32)
        P.dma(cosT, rope_cos_d, reads=[], writes=["cosT"])
        P.dma(sinT, rope_sin_d, reads=[], writes=["sinT"])
        qT = AR.alloc([8, T], BF16)
        kT = AR.alloc([4, T], BF16)
        vaug = AR.alloc([NB, 4, 65], BF16)
        wst = AR.alloc([2, KC, 512], BF16)
        tmp1 = AR.alloc([512], F32)
        tmp2 = AR.alloc([512], F32)
        esink = AR.alloc([16], F32)
        Et = AR.alloc([2, 128], BF16)
        att = AR.alloc([D], BF16)
        attT = AR.alloc([KC, 128], BF16)
        rden = AR.alloc([4], F32)
        AR_attT[0] = attT
        P.dma(esink, od_sink_d[j:j + 1, :].to_broadcast([128, 16]), reads=[], writes=["esink"])
        _act(P, esink, esink, AF.Exp, ["esink"], ["esink"])
        _memset(P, "pool", vaug[:, :, :, 64:65], 1.0, ["vaug"])
        wv = od_w_in_d[j].rearrange("(k p) n -> p k n", p=128)
        wsw = od_w_sw_d[j].rearrange("(k p) n -> p k n", p=128)
        TG = [(t0, min(512, T - t0)) for t0 in range(0, T, 512)]
        nld = [0]

        def loadw(src, c0, ncol):
            b = nld[0] % 2
            nld[0] += 1
            load_cast(wst[:, b, :, 0:ncol], src[:, :, c0:c0 + ncol], ("wst", b))
            return b

        for ci in range(12):
            dst = qT[:, ci, :] if ci < 8 else kT[:, ci - 8, :]
            dname = ("qT", ci) if ci < 8 else ("kT", ci - 8)
            if ci % 4 == 0:
                ncol = 512 if ci < 8 else 256
                b0 = loadw(wv, ci * 128 if ci < 8 else 1024, ncol)
                b1 = loadw(wsw, ci * 128 if ci < 8 else 1024, ncol)
            if ci < 8:
                parts = [(0, 128, (ci % 4) * 128, 128)]
            else:
                parts = [(0, 64, (ci - 8) * 64, 64), (64, 128, (ci - 8) * 64, 64)]
            for (t0, tn) in TG:
                tl = max(0, min(tn, TL - t0))
                for (p0, p1, cc, cn) in parts:
                    for kc in range(KC):
                        _mm(P, ps[0][p0:p1, 0:tn], wst[:, b0, kc, cc:cc + cn], xT[:, kc, t0:t0 + tn], kc == 0, kc == KC - 1,
                            [("wst", b0)] + XTALL, [PSr(0)])
                    if tl > 0:
                        for kc in range(KC):
                            _mm(P, ps[1][p0:p1, 0:tl], wst[:, b1, kc, cc:cc + cn], xT[:, kc, t0:t0 + tl], kc == 0, kc == KC - 1,
                                [("wst", b1)] + XTALL, [PSr(1)])
                if tl > 0:
                    _tt(P, "dve", tmp1[:, 0:tl], ps[0][:, 0:tl], cosT[:, t0:t0 + tl], ALU.mult, [PSr(0), "cosT"], ["tmp1"])
                    _tt(P, "dve", tmp2[:, 0:tl], ps[1][:, 0:tl], sinT[:, t0:t0 + tl], ALU.mult, [PSr(1), "sinT"], ["tmp2"])
                    _tt(P, "dve", dst[:, t0:t0 + tl], tmp1[:, 0:tl], tmp2[:, 0:tl], ALU.add, ["tmp1", "tmp2"], [dname])
                if tl < tn:
                    _cp(P, "act", dst[:, t0 + tl:t0 + tn], ps[0][:, tl:tn], [PSr(0)], [dname])
        bv = loadw(wv, 1280, 256)
        for blk in range(NB):
            for kc in range(KC):
                _mm(P, ps[2][:, 0:256], xT[:, kc, blk * 128:(blk + 1) * 128], wst[:, bv, kc, 0:256], kc == 0, kc == KC - 1,
                    [("wst", bv)] + XTALL, [PSr(2)])
            _cp(P, "act", vaug[:, blk, :, 0:64], ps[2][:, 0:256].rearrange("p (h d) -> p h d", h=4), [PSr(2)], ["vaug"])
        wo = AR.alloc([KC, D], BF16)
        load_cast(wo, od_w_out_d[j].rearrange("(k p) n -> p k n", p=128), "wo")
        qblocks = list(range(NLB)) + (list(range(NLB, NB)) if need_ctx else [])
        nE = [0]
        for qb in qblocks:
            if qb < NLB:
                keys = [(kb, None) for kb in range(NLB, NB)]
                if qb > 0:
                    keys.append((qb - 1, tri_ge))
                keys.append((qb, None))
                if qb + 1 < NLB:
                    keys.append((qb + 1, tri_le))
            else:
                keys = [(kb, None) for kb in range(NLB, NB)]
            for g in range(4):
                for hh in range(4):
                    hq = g * 4 + hh
                    qa = qT[(hq % 2) * 64:(hq % 2) * 64 + 64, hq // 2, qb * 128:(qb + 1) * 128]
                    for ki, (kb, mask) in enumerate(keys):
                        ka = kT[(hq % 2) * 64:(hq % 2) * 64 + 64, g, kb * 128:(kb + 1) * 128]
                        eb = nE[0] % 2
                        nE[0] += 1
                        attend_step(qa, ka, vaug[:, kb, g, :], ps[4 + g % 2][:, hh * 65:hh * 65 + 65], PSr(4 + g % 2),
                                    ki == 0, ki == len(keys) - 1, AF.Exp, 0.125, mask,
                                    ("qT", hq // 2), ("kT", g), "vaug", 2 + eb, Et[:, eb, :], ("Et", eb))
                po = ps[4 + g % 2][:, 0:260].rearrange("p (h d) -> p h d", h=4)
                _tt(P, "dve", rden, po[:, :, 64], esink[:, g * 4:(g + 1) * 4], ALU.add, [PSr(4 + g % 2), "esink"], ["rden"])
                P._add("dve", lambda e: e.reciprocal(out=rden, in_=rden), ["rden"], ["rden"])
                _tt(P, "dve", att[:, g * 256:(g + 1) * 256].rearrange("p (h d) -> p h d", h=4), po[:, :, 0:64],
                    rden.unsqueeze(2).to_broadcast([128, 4, 64]), ALU.mult, [PSr(4 + g % 2), "rden"], ["att"])
            out_proj_residual(qb, att, wo, after_block)
        P.barrier()

    def out_proj_residual(qb, att, wo, after_block):
        attT = AR_attT[0]
        lc = 0 if qb < NLB else 1
        for kc in range(KC):
            _tr(P, psb[6][:, kc * 128:(kc + 1) * 128], att[:, kc * 128:(kc + 1) * 128], identb[:], ["att", "identb"], [PSr(6)])
        _cp(P, "act", attT, psb[6][:, 0:D].rearrange("p (k t) -> p k t", k=KC), [PSr(6)], ["attT"])
        load_h(qb, hA, "hA")
        for half in range(2):
            for kc in range(KC):
                _mm(P, ps[0][:, :], attT[:, kc, :], wo[:, kc, half * 512:(half + 1) * 512], kc == 0, kc == KC - 1,
                    ["attT", "wo"], [PSr(0)])
            _tt(P, "dve", hB[:, half * 512:(half + 1) * 512], ps[0][:, :], gates[:, lc, half * 512:(half + 1) * 512], ALU.mult,
                [PSr(0), "gates"], ["hB"])
            _tt(P, "dve", hA[:, half * 512:(half + 1) * 512], hA[:, half * 512:(half + 1) * 512],
                hB[:, half * 512:(half + 1) * 512], ALU.add, ["hA", "hB"], ["hA"])
        after_block(qb, hA, "hA")

    AR_attT = [None]


    def even_mixer(l, j, need_ctx, after_block, lam_init):
        AR.reset()
        wv = ev_w_in_d[j].rearrange("(k p) n -> p k n", p=128)
        wsw = ev_w_sw_d[j].rearrange("(k p) n -> p k n", p=128)
        TG = [(t0, min(512, T - t0)) for t0 in range(0, T, 512)]
        wst = AR.alloc([2, KC, 512], BF16)
        nld = [0]

        def loadw(src, c0, ncol):
            b = nld[0] % 2
            nld[0] += 1
            load_cast(wst[:, b, :, 0:ncol], src[:, :, c0:c0 + ncol], ("wst", b))
            return b
        og = AR.alloc([NB, 512], BF16)
        mlout = og
        att = AR.alloc([D], BF16)
        attT = AR.alloc([KC, 128], BF16)
        AR_attT[0] = attT
        Et = AR.alloc([2, 128], BF16)
        mlg = AR.alloc([512], F32)
        dag = AR.alloc([512], F32)
        P.dma(mlg, ml_g_d[j:j + 1, :].to_broadcast([128, 512]), reads=[], writes=["mlg"])
        P.dma(dag, da_g_d[j:j + 1, :].to_broadcast([128, 512]), reads=[], writes=["dag"])
        hsum = AR.alloc([512], F32)
        sq = AR.alloc([512], F32)
        s4 = AR.alloc([4], F32)
        s1c = AR.alloc([8], F32)
        mark = AR.off
        qk = AR.alloc([4, T], BF16)
        vml = AR.alloc([NB, 4, 129], BF16)
        GT = AR.alloc([NB, 16], F32)
        PFT = AR.alloc([NB, 16], F32)
        PBT = AR.alloc([NB, 16], F32)
        prefF = AR.alloc([NB, 16], F32)
        prefB = AR.alloc([NB, 16], F32)
        imP = AR.alloc([2, 4, NB], F32)
        KF = AR.alloc([8, NB, NB], F32)
        afac = AR.alloc([2, 4, NB], F32)
        cw = AR.alloc([4, 3], F32)
        cb = AR.alloc([4], F32)
        gb = AR.alloc([1], F32, parts=16)
        tot = AR.alloc([1], F32, parts=16)
        mark2 = AR.off
        bufA = AR.alloc([T], F32, parts=16)
        bufB = AR.alloc([T], F32, parts=16)
        for t_ in range(3):
            P.dma(cw[:, :, t_], conv_w_d[j][t_:t_ + 1, :].rearrange("o (c p) -> p (o c)", p=128), reads=[], writes=["cw"],
                  allow_slow_non_contiguous=True)
        P.dma(cb, conv_b_d[j:j + 1, :].rearrange("o (c p) -> p (o c)", p=128), reads=[], writes=["cb"], allow_slow_non_contiguous=True)
        P.dma(gb, gate_b_d[j:j + 1, :].rearrange("o g -> g o"), reads=[], writes=["gb"], allow_slow_non_contiguous=True)
        _memset(P, "pool", vml[:, :, :, 128:129], 1.0, ["vml"])
        bg = loadw(wv, 1536, 16)
        for (t0, tn) in TG:
            for kc in range(KC):
                _mm(P, ps[3][0:16, 0:tn], wst[:, bg, kc, 0:16], xT[:, kc, t0:t0 + tn], kc == 0, kc == KC - 1,
                    [("wst", bg)] + XTALL, [PSr(3)])
            _ts(P, "dve", bufA[:, t0:t0 + tn], ps[3][0:16, 0:tn], gb[:, 0:1], None, ALU.add, None, [PSr(3), "gb"], ["bufA"])

        def rows_to_tok(src, sname, dstT, dname):
            for blk in range(NB):
                _tr(P, ps[3][:, blk * 16:(blk + 1) * 16], src[:, blk * 128:(blk + 1) * 128], identf[0:16, 0:16],
                    [sname, "identf"], [PSr(3)])
            _cp(P, "dve", dstT, ps[3][:, 0:NB * 16].rearrange("p (b g) -> p b g", b=NB), [PSr(3)], [dname])
        rows_to_tok(bufA, "bufA", GT, "GT")
        _act(P, bufA, bufA, AF.Exp, ["bufA"], ["bufA"], scale=-1.0)
        _act(P, bufA, bufA, AF.Ln, ["bufA"], ["bufA"], bias=1.0)
        _ts(P, "dve", bufA, bufA, -0.5, None, ALU.mult, None, ["bufA"], ["bufA"])
        P._add("dve", lambda e: e.tensor_tensor_scan(out=bufB[:, TL:T], data0=bufA[:, TL:T], data1=bufA[:, TL:T], initial=0.0,
                                                     op0=ALU.add, op1=ALU.add), ["bufA"], ["bufB"])
        _cp(P, "dve", tot, bufB[:, T - 1:T], ["bufB"], ["tot"])
        P._add("dve", lambda e: e.tensor_tensor_scan(out=bufB[:, 0:TL], data0=bufA[:, 0:TL], data1=bufA[:, 0:TL],
                                                     initial=tot[:, 0:1], op0=ALU.add, op1=ALU.add),
               ["bufA", "bufB", "tot"], ["bufB"])
        rows_to_tok(bufB, "bufB", PFT, "PFT")
        P._add("dve", lambda e: e.tensor_tensor_scan(out=bufB[:, 0:T], data0=bufA[:, 0:T], data1=bufA[:, 0:T], initial=0.0,
                                                     op0=ALU.add, op1=ALU.add), ["bufA", "bufB"], ["bufB"])
        _cp(P, "dve", tot, bufB[:, T - 1:T], ["bufB"], ["tot"])
        _stt(P, "dve", bufB, bufB, -1.0, bufA, ALU.mult, ALU.add, ["bufB", "bufA"], ["bufB"])
        _stt(P, "dve", bufB, bufB, tot[:, 0:1], bufA, ALU.add, ALU.add, ["bufB", "bufA", "tot"], ["bufB"])
        rows_to_tok(bufB, "bufB", PBT, "PBT")
        for (src, sname, dst, dname) in ((PFT, "PFT", prefF, "prefF"), (PBT, "PBT", prefB, "prefB")):
            _mm(P, ps[3][:, 0:NB * 16], sel[:, 0, :], src[0:2].rearrange("p b g -> p (b g)"), True, True, ["sel", sname], [PSr(3)])
            _cp(P, "dve", dst, ps[3][:, 0:NB * 16].rearrange("p (b g) -> p b g", b=NB), [PSr(3)], [dname])
        for d_ in range(2):
            PT_ = PFT if d_ == 0 else PBT
            pr_ = prefF if d_ == 0 else prefB
            for h in range(4):
                col = d_ * 8 + 4 + h
                _tt(P, "dve", imP[:, d_, h, :], GT[:, :, d_ * 8 + h], PT_[:, :, col], ALU.subtract,
                    ["GT", "PFT", "PBT"], ["imP"])
                for kb in range(NB):
                    _act(P, KF[:, d_ * 4 + h, kb, :], pr_[:, :, col], AF.Exp, ["prefF", "prefB", "imP"], ["KF"],
                         bias=imP[:, d_, h, kb:kb + 1])
                _tt(P, "dve", afac[:, d_, h, :], PT_[:, :, col], pr_[:, :, col], ALU.subtract,
                    ["PFT", "PBT", "prefF", "prefB"], ["afac"])
        _act(P, afac, afac, AF.Exp, ["afac"], ["afac"])
        _ts(P, "dve", afac, afac, 0.125, None, ALU.mult, None, ["afac"], ["afac"])
        P.barrier()
        AR.off = mark2
        pre = AR.alloc([T], F32)
        cacc = AR.alloc([T], F32)
        bq = loadw(wv, 0, 512)
        for c in range(4):
            for (t0, tn) in TG:
                for kc in range(KC):
                    _mm(P, ps[0][:, 0:tn], wst[:, bq, kc, c * 128:(c + 1) * 128], xT[:, kc, t0:t0 + tn], kc == 0, kc == KC - 1,
                        [("wst", bq)] + XTALL, [PSr(0)])
                _cp(P, "act", pre[:, t0:t0 + tn], ps[0][:, 0:tn], [PSr(0)], ["pre"])
            _ts(P, "dve", cacc, pre, cw[:, c, 1:2], cb[:, c:c + 1], ALU.mult, ALU.add, ["pre", "cw", "cb"], ["cacc"])
            for (a0, a1) in ((0, TL), (TL, T)):
                _stt(P, "dve", cacc[:, a0 + 1:a1], pre[:, a0:a1 - 1], cw[:, c, 0:1], cacc[:, a0 + 1:a1], ALU.mult, ALU.add,
                     ["pre", "cw", "cacc"], ["cacc"])
                _stt(P, "dve", cacc[:, a0:a1 - 1], pre[:, a0 + 1:a1], cw[:, c, 2:3], cacc[:, a0:a1 - 1], ALU.mult, ALU.add,
                     ["pre", "cw", "cacc"], ["cacc"])
            _act(P, qk[:, c, :], cacc, AF.Silu, ["cacc"], [("qk", c)])
        bvv = loadw(wv, 512, 512)
        bo = loadw(wv, 1024, 512)
        for blk in range(NB):
            for kc in range(KC):
                _mm(P, ps[1][:, :], xT[:, kc, blk * 128:(blk + 1) * 128], wst[:, bvv, kc, :], kc == 0, kc == KC - 1,
                    [("wst", bvv)] + XTALL, [PSr(1)])
            _cp(P, "act", vml[:, blk, :, 0:128], ps[1][:, :].rearrange("p (h d) -> p h d", h=4), [PSr(1)], ["vml"])
            for kc in range(KC):
                _mm(P, ps[2][:, :], xT[:, kc, blk * 128:(blk + 1) * 128], wst[:, bo, kc, :], kc == 0, kc == KC - 1,
                    [("wst", bo)] + XTALL, [PSr(2)])
            _act(P, og[:, blk, :], ps[2][:, :], AF.Sigmoid, [PSr(2)], ["og"])
        qblocks = list(range(NLB)) + (list(range(NLB, NB)) if need_ctx else [])
        nE = [0]
        for qb in qblocks:
            for h in range(4):
                qa = qk[(h % 2) * 64:(h % 2) * 64 + 64, h // 2, qb * 128:(qb + 1) * 128]
                for d_ in range(2):
                    if qb < NLB:
                        keys = [(kb, None) for kb in range(NLB, NB)]
                        keys += [(kb, None) for kb in (range(0, qb) if d_ == 0 else range(qb + 1, NLB))]
                    else:
                        keys = [(kb, None) for kb in (range(NLB, qb) if d_ == 0 else range(qb + 1, NB))]
                    keys.append((qb, tri_le if d_ == 0 else tri_ge))
                    pso = 4 + d_
                    for ki, (kb, mask) in enumerate(keys):
                        ka = qk[(h % 2) * 64:(h % 2) * 64 + 64, 2 + h // 2, kb * 128:(kb + 1) * 128]
                        eb = nE[0] % 2
                        nE[0] += 1
                        attend_step(qa, ka, vml[:, kb, h, :], ps[pso][:, 0:129], PSr(pso), ki == 0, ki == len(keys) - 1,
                                    AF.Identity, (KF[:, d_ * 4 + h, kb, qb:qb + 1], "KF"), mask,
                                    ("qk", h // 2), ("qk", 2 + h // 2), "vml", 2 + eb, Et[:, eb, :], ("Et", eb))
                    a_ = afac[:, d_, h, qb:qb + 1]
                    _tt(P, "dve", s1c[:, d_:d_ + 1], ps[pso][:, 128:129], a_, ALU.mult, [PSr(pso), "afac"], ["s1c"])
                    _stt(P, "dve", s1c[:, 2 + d_:3 + d_], s1c[:, d_:d_ + 1], -1.0, s1c[:, d_:d_ + 1], ALU.mult, ALU.max, ["s1c"], ["s1c"])
                    _ts(P, "dve", s1c[:, d_:d_ + 1], s1c[:, 2 + d_:3 + d_], 1.0, None, ALU.max, None, ["s1c"], ["s1c"])
                    P._add("dve", lambda e, d_=d_: e.reciprocal(out=s1c[:, d_:d_ + 1], in_=s1c[:, d_:d_ + 1]), ["s1c"], ["s1c"])
                    _tt(P, "dve", s1c[:, d_:d_ + 1], s1c[:, d_:d_ + 1], a_, ALU.mult, ["s1c", "afac"], ["s1c"])
                _ts(P, "dve", hsum[:, h * 128:(h + 1) * 128], ps[4][:, 0:128], s1c[:, 0:1], None, ALU.mult, None,
                    [PSr(4), "s1c"], ["hsum"])
                _stt(P, "dve", hsum[:, h * 128:(h + 1) * 128], ps[5][:, 0:128], s1c[:, 1:2], hsum[:, h * 128:(h + 1) * 128],
                     ALU.mult, ALU.add, [PSr(5), "s1c", "hsum"], ["hsum"])
            headnorm(hsum, sq, s4, mlg, 1.0, mlout[:, qb, :], "og", og[:, qb, :])
        P.barrier()
        AR.off = mark
        qda = AR.alloc([4, T], BF16)
        kda = AR.alloc([4, T], BF16)
        vda = AR.alloc([NB, 4, 129], BF16)
        lamt = AR.alloc([256], F32)
        lam = AR.alloc([4], F32)
        r2 = AR.alloc([2], F32)
        o1 = AR.alloc([128], F32)
        mark3 = AR.off
        cosT = AR.alloc([TL], F32)
        sinT = AR.alloc([TL], F32)
        tmp1 = AR.alloc([512], F32)
        tmp2 = AR.alloc([512], F32)
        P.dma(cosT, rope_cos_d, reads=[], writes=["cosT"])
        P.dma(sinT, rope_sin_d, reads=[], writes=["sinT"])
        P.dma(lamt, da_lam_d[j:j + 1, :].to_broadcast([128, 256]), reads=[], writes=["lamt"])
        l4 = lamt.rearrange("p (a b) -> p a b", a=4)
        _tt(P, "dve", tmp1[:, 0:64], l4[:, 0, :], l4[:, 1, :], ALU.mult, ["lamt"], ["tmp1"])
        _tt(P, "dve", tmp1[:, 64:128], l4[:, 2, :], l4[:, 3, :], ALU.mult, ["lamt"], ["tmp1"])
        P._add("dve", lambda e: e.tensor_reduce(out=lam[:, 0:2], in_=tmp1[:, 0:128].rearrange("p (a b) -> p a b", a=2), axis=AX.X,
                                                op=ALU.add), ["tmp1"], ["lam"])
        _act(P, lam[:, 0:2], lam[:, 0:2], AF.Exp, ["lam"], ["lam"])
        _tt(P, "dve", lam[:, 2:3], lam[:, 1:2], lam[:, 0:1], ALU.subtract, ["lam"], ["lam"])
        _ts(P, "dve", lam[:, 2:3], lam[:, 2:3], -lam_init, None, ALU.add, None, ["lam"], ["lam"])
        _memset(P, "pool", vda[:, :, :, 128:129], 1.0, ["vda"])
        for ci in range(8):
            dst = qda[:, ci, :] if ci < 4 else kda[:, ci - 4, :]
            dname = ("qda", ci) if ci < 4 else ("kda", ci - 4)
            if ci % 4 == 0:
                b0 = loadw(wv, 1552 + ci * 128, 512)
                b1 = loadw(wsw, ci * 128, 512)
            cc = (ci % 4) * 128
            for (t0, tn) in TG:
                tl = max(0, min(tn, TL - t0))
                for kc in range(KC):
                    _mm(P, ps[0][:, 0:tn], wst[:, b0, kc, cc:cc + 128], xT[:, kc, t0:t0 + tn], kc == 0, kc == KC - 1,
                        [("wst", b0)] + XTALL, [PSr(0)])
                if tl > 0:
                    for kc in range(KC):
                        _mm(P, ps[1][:, 0:tl], wst[:, b1, kc, cc:cc + 128], xT[:, kc, t0:t0 + tl], kc == 0, kc == KC - 1,
                            [("wst", b1)] + XTALL, [PSr(1)])
                    _tt(P, "dve", tmp1[:, 0:tl], ps[0][:, 0:tl], cosT[:, t0:t0 + tl], ALU.mult, [PSr(0), "cosT"], ["tmp1"])
                    _tt(P, "dve", tmp2[:, 0:tl], ps[1][:, 0:tl], sinT[:, t0:t0 + tl], ALU.mult, [PSr(1), "sinT"], ["tmp2"])
                    _tt(P, "dve", dst[:, t0:t0 + tl], tmp1[:, 0:tl], tmp2[:, 0:tl], ALU.add, ["tmp1", "tmp2"], [dname])
                if tl < tn:
                    _cp(P, "act", dst[:, t0 + tl:t0 + tn], ps[0][:, tl:tn], [PSr(0)], [dname])
        bdv = loadw(wv, 2576, 512)
        for blk in range(NB):
            for kc in range(KC):
                _mm(P, ps[2][:, :], xT[:, kc, blk * 128:(blk + 1) * 128], wst[:, bdv, kc, :], kc == 0, kc == KC - 1,
                    [("wst", bdv)] + XTALL, [PSr(2)])
            _cp(P, "act", vda[:, blk, :, 0:128], ps[2][:, :].rearrange("p (h d) -> p h d", h=4), [PSr(2)], ["vda"])
        P.barrier()
        AR.off = mark3
        wo = AR.alloc([KC, D], BF16)
        load_cast(wo, ev_w_out_d[j].rearrange("(k p) n -> p k n", p=128), "wo")
        for qb in qblocks:
            keys = list(range(NLB, NB)) + (list(range(NLB)) if qb < NLB else [])
            for h in range(4):
                for m in range(2):
                    qa = qda[m * 64:(m + 1) * 64, h, qb * 128:(qb + 1) * 128]
                    pso = 4 + m
                    for ki, kb in enumerate(keys):
                        ka = kda[m * 64:(m + 1) * 64, h, kb * 128:(kb + 1) * 128]
                        eb = nE[0] % 2
                        nE[0] += 1
                        attend_step(qa, ka, vda[:, kb, h, :], ps[pso][:, 0:129], PSr(pso), ki == 0, ki == len(keys) - 1,
                                    AF.Exp, 0.125, None, ("qda", h), ("kda", h), "vda", 2 + eb, Et[:, eb, :], ("Et", eb))
                    P._add("dve", lambda e, m=m, pso=pso: e.reciprocal(out=r2[:, m:m + 1], in_=ps[pso][:, 128:129]),
                           [PSr(pso)], ["r2"])
                _tt(P, "dve", r2[:, 1:2], r2[:, 1:2], lam[:, 2:3], ALU.mult, ["r2", "lam"], ["r2"])
                _ts(P, "dve", o1, ps[4][:, 0:128], r2[:, 0:1], None, ALU.mult, None, [PSr(4), "r2"], ["o1"])
                _stt(P, "dve", hsum[:, h * 128:(h + 1) * 128], ps[5][:, 0:128], r2[:, 1:2], o1, ALU.mult, ALU.add,
                     [PSr(5), "r2", "o1"], ["hsum"])
            headnorm(hsum, sq, s4, dag, 1.0 - lam_init, att[:, 512:1024], "att", None)
            _cp(P, "pool", att[:, 0:512], mlout[:, qb, :], ["og"], ["att"])
            out_proj_residual(qb, att, wo, after_block)
        P.barrier()

    def headnorm(hsum, sq, s4, gtile, mult, dst, dname, gate):
        h3 = hsum.rearrange("p (h d) -> p h d", h=4)
        _tt(P, "dve", sq, hsum, hsum, ALU.mult, ["hsum"], ["sq"])
        P._add("dve", lambda e: e.tensor_reduce(out=s4, in_=sq.rearrange("p (h d) -> p h d", h=4), axis=AX.X, op=ALU.add),
               ["sq"], ["s4"])
        _ts(P, "dve", s4, s4, 1.0 / 128, EPS, ALU.mult, ALU.add, ["s4"], ["s4"])
        _act(P, s4, s4, AF.Sqrt, ["s4"], ["s4"])
        P._add("dve", lambda e: e.reciprocal(out=s4, in_=s4), ["s4"], ["s4"])
        _tt(P, "dve", sq.rearrange("p (h d) -> p h d", h=4), h3, s4.unsqueeze(2).to_broadcast([128, 4, 128]), ALU.mult,
            ["hsum", "s4"], ["sq"])
        if gate is None:
            _stt(P, "dve", dst, sq, mult, gtile, ALU.mult, ALU.mult, ["sq"], [dname])
        else:
            _stt(P, "dve", sq, sq, mult, gtile, ALU.mult, ALU.mult, ["sq"], ["sq"])
            _tt(P, "dve", dst, sq, gate, ALU.mult, ["sq", "og"], [dname])

    def after_mixer(l):
        def f(blk, tile, tname):
            store_h(blk, tile, tname)
            norm_to_xT(blk, tile, tname, 1)
        return f

    def after_peer(l):
        last = (l == DEPTH - 1)

        def f(blk, tile, tname):
            if not last:
                store_h(blk, tile, tname)
            elif blk < NLB:
                final_norm(blk, tile, tname)
        return f

    fg = gates[:, 0, :]

    def final_norm(blk, tile, tname):
        ss = small[:, 2:3]
        rs = small[:, 3:4]
        _act(P, hC[:], tile[:], AF.Square, [tname], ["hC", "ss2"], accum_out=ss)
        _ts(P, "dve", rs, ss, 1.0 / D, EPS, ALU.mult, ALU.add, ["ss2"], ["rs2"])
        _act(P, rs, rs, AF.Sqrt, ["rs2"], ["rs2"])
        P._add("dve", lambda e: e.reciprocal(out=rs, in_=rs), ["rs2"], ["rs2"])
        _stt(P, "dve", hB[:], tile[:], rs, fg, ALU.mult, ALU.mult, [tname, "rs2", "gates"], ["hB"])
        P.dma(out_d[blk * 128:(blk + 1) * 128, :], hB[:], reads=["hB"], writes=["out"])

    import math
    for blk in range(NB):
        pass
    for l in range(DEPTH):
        need_ctx = l < DEPTH - 1
        modvec(l)
        for blk in range(NB):
            load_h(blk, hA, "hA")
            norm_to_xT(blk, hA, "hA", 0)
        P.barrier()
        if l % 2 == 0:
            even_mixer(l, l // 2, need_ctx, after_mixer(l), 0.8 - 0.6 * math.exp(-0.3 * l))
        else:
            odd_mixer(l, l // 2, need_ctx, after_mixer(l))
        pblocks = list(range(NLB)) + (list(range(NLB, NB)) if need_ctx else [])
        if l == DEPTH - 1:
            P.dma(fg, final_g_d.to_broadcast([128, D]), reads=["gates"], writes=["gates"])
        peer(l, pblocks, after_peer(l))
    P.finish(outputs=["out"])
    return nc, P


def _rope_tables(n_tok, grid_w=64, dim=64, theta=10000.0):
    rows = n_tok // grid_w
    r = np.repeat(np.arange(rows, dtype=np.float32), grid_w)
    col = np.tile(np.arange(grid_w, dtype=np.float32), rows)
    nf = dim // 4
    inv = (theta ** (-np.arange(nf, dtype=np.float32) / nf)).astype(np.float32)
    ang = np.concatenate([r[:, None] * inv, col[:, None] * inv], axis=-1).astype(np.float32)
    cos = np.cos(ang).astype(np.float32).T
    sin = np.sin(ang).astype(np.float32).T
    cos64 = np.concatenate([cos, cos], axis=0)
    sin64 = np.concatenate([-sin, sin], axis=0)
    return (np.ascontiguousarray(np.concatenate([cos64, cos64], axis=0)),
            np.ascontiguousarray(np.concatenate([sin64, sin64], axis=0)))


def _swap_cols(w):
    n = w.shape[-1]
    w4 = w.reshape(w.shape[:-1] + (n // 64, 2, 32))
    return np.ascontiguousarray(w4[..., ::-1, :].reshape(w.shape))


_PROG_CACHE = {}


def prepare_inputs(inp, NLB, NCB, DEPTH):
    f = lambda a: np.ascontiguousarray(np.asarray(a, dtype=np.float32))
    bsz = inp["x"].shape[0]
    NE = (DEPTH + 1) // 2
    NO = max(DEPTH // 2, 1)
    cos, sin = _rope_tables(NLB * 128)
    shared = {
        "mod_w": f(inp["mod_w"]), "mod_b": f(inp["mod_b"]), "norm1_g": f(inp["norm1_g"]), "norm2_g": f(inp["norm2_g"]),
        "ev_w_in": f(inp["ev_w_in"]), "ev_w_sw": _swap_cols(f(inp["ev_w_in"])[:, :, 1552:2576]),
        "ev_ml_conv_w": f(inp["ev_ml_conv_w"]), "ev_ml_conv_b": f(inp["ev_ml_conv_b"]), "ev_ml_gate_b": f(inp["ev_ml_gate_b"]),
        "ev_ml_norm_g": f(inp["ev_ml_norm_g"]), "ev_da_lam": f(inp["ev_da_lam"]).reshape(NE, 256),
        "ev_da_norm_g": f(inp["ev_da_norm_g"]), "ev_w_out": f(inp["ev_w_out"]),
        "pr_w_q": f(inp["pr_w_q"]),
        "pr_keysT": np.ascontiguousarray(f(inp["pr_keys"]).transpose(0, 1, 2, 4, 3).reshape(DEPTH, 16, 128, 128)),
        "pr_uT": np.ascontiguousarray(f(inp["pr_u"]).transpose(0, 2, 1)), "pr_v": f(inp["pr_v"]),
        "final_g": f(inp["final_g"]).reshape(1, -1), "rope_cos": cos, "rope_sin": sin,
    }
    if DEPTH // 2 > 0:
        shared.update({"od_w_in": f(inp["od_w_in"]), "od_w_sw": _swap_cols(f(inp["od_w_in"])[:, :, 0:1280]),
                       "od_sink": f(inp["od_sink"]), "od_w_out": f(inp["od_w_out"])})
    else:
        shared.update({"od_w_in": np.zeros((NO, 1024, 1536), np.float32), "od_w_sw": np.zeros((NO, 1024, 1280), np.float32),
                       "od_sink": np.zeros((NO, 16), np.float32), "od_w_out": np.zeros((NO, 1024, 1024), np.float32)})
    maps = []
    for b in range(bsz):
        m = dict(shared)
        m["x"] = f(inp["x"][b])
        m["ctx"] = f(inp["ctx"][b])
        m["c2"] = np.ascontiguousarray(np.stack([f(inp["c"][b]), f(inp["c_ctx"])], axis=0))
        maps.append(m)
    return maps


def kernel(**inputs):
    NLB, NCB, DEPTH = 16, 2, 4
    key = (NLB, NCB, DEPTH)
    if key not in _PROG_CACHE:
        _PROG_CACHE[key] = build_program(NLB, NCB, DEPTH)[0]
    nc = _PROG_CACHE[key]
    maps = prepare_inputs(inputs, NLB, NCB, DEPTH)
    res = run_bass_kernel_spmd(nc, maps, core_ids=list(range(len(maps))))
    return np.stack([np.asarray(r["out"], dtype=np.float32) for r in res.results], axis=0)
```

```python
from contextlib import ExitStack
import numpy as np
import concourse.bass as bass
import concourse.mybir as mybir
from concourse.bass_utils import run_bass_kernel_spmd

F32 = mybir.dt.float32
BF16 = mybir.dt.bfloat16
ALU = mybir.AluOpType
AF = mybir.ActivationFunctionType
AX = mybir.AxisListType

N_DMA_SEMS = 24


class _Op:
    __slots__ = ("eng", "fn", "reads", "writes", "waits", "signal", "sem", "val", "idx", "kind")

    def __init__(self, eng, fn, reads, writes, kind="op"):
        self.eng = eng
        self.fn = fn
        self.reads = tuple(reads)
        self.writes = tuple(writes)
        self.waits = []
        self.signal = False
        self.sem = None
        self.val = 0
        self.kind = kind


class Prog:
    ENGS = ("pe", "act", "dve", "pool", "sp")

    def __init__(self, nc):
        self.nc = nc
        self.ops = []
        self.stack = ExitStack()
        self._n = 0

    def sb(self, name, shape, dtype):
        return self.stack.enter_context(self.nc.sbuf_tensor(name, list(shape), dtype))

    def ps(self, name, shape, dtype=F32):
        return self.stack.enter_context(self.nc.psum_tensor(name, list(shape), dtype))

    def _add(self, eng, fn, reads, writes, kind="op"):
        op = _Op(eng, fn, reads, writes, kind)
        op.idx = len(self.ops)
        self.ops.append(op)
        return op

    def pe(self, fn, reads=(), writes=()):
        return self._add("pe", fn, reads, writes)

    def act(self, fn, reads=(), writes=()):
        return self._add("act", fn, reads, writes)

    def dve(self, fn, reads=(), writes=()):
        return self._add("dve", fn, reads, writes)

    def pool(self, fn, reads=(), writes=()):
        return self._add("pool", fn, reads, writes)

    def barrier(self):
        for e in self.ENGS:
            self._add(e, None, [], [], kind="barrier")

    def dma(self, out, in_, reads=(), writes=(), eng="sp", **kw):
        return self._add(eng, lambda e: e.dma_start(out=out, in_=in_, **kw), reads, writes, kind="dma")

    def finish(self, outputs=()):
        nc = self.nc
        ops = self.ops
        fence = self._add("sp", None, list(outputs), [], kind="fence")
        last_w = {}
        readers = {}
        deps_of = []
        last_on_eng = {}
        all_dmas = []
        for op in ops:
            deps = {}
            if op.kind == "barrier":
                for p in last_on_eng.values():
                    deps[p] = True
                for p in all_dmas:
                    deps[p] = True
                deps_of.append(deps)
                continue
            if op.kind == "dma":
                all_dmas.append(op.idx)
            elif op.kind == "op":
                last_on_eng[op.eng] = op.idx
            for r in op.reads:
                p = last_w.get(r)
                if p is not None:
                    deps[p] = True
            for w in op.writes:
                p = last_w.get(w)
                if p is not None and p not in deps:
                    deps[p] = False
                for rd in readers.get(w, ()):
                    if rd not in deps:
                        deps[rd] = False
            deps.pop(op.idx, None)
            for r in op.reads:
                readers.setdefault(r, []).append(op.idx)
            for w in op.writes:
                last_w[w] = op.idx
                readers[w] = []
            deps_of.append(deps)
        for op, deps in zip(ops, deps_of):
            keep = []
            for p, raw in deps.items():
                pop = ops[p]
                if pop.kind == "dma":
                    keep.append(p)
                elif pop.eng == op.eng:
                    if op.eng != "pe":
                        keep.append(p)
                else:
                    keep.append(p)
            op.waits = sorted(keep)
            for p in keep:
                ops[p].signal = True
        sems = {e: self.stack.enter_context(nc.semaphore("s_" + e)) for e in ("pe", "act", "dve", "pool")}
        dsems = [self.stack.enter_context(nc.semaphore("d%d" % i)) for i in range(N_DMA_SEMS)]
        cnt = {e: 0 for e in sems}
        ndma = 0
        dma_prev = {}
        for op in ops:
            if op.kind == "dma":
                k = ndma % N_DMA_SEMS
                op.sem = dsems[k]
                op.val = 16 * (ndma // N_DMA_SEMS + 1)
                prev = dma_prev.get(k)
                if prev is not None and prev.idx not in op.waits:
                    op.waits.append(prev.idx)
                dma_prev[k] = op
                op.signal = True
                ndma += 1
            elif op.kind == "op" and op.signal:
                cnt[op.eng] += 1
                op.sem = sems[op.eng]
                op.val = cnt[op.eng]
        per_eng = {e: [op for op in ops if op.eng == e] for e in self.ENGS}
        self.stats = {e: len(v) for e, v in per_eng.items()}

        def emit(engname, eng):
            waited = {}
            for op in per_eng[engname]:
                need = {}
                for p in op.waits:
                    pop = ops[p]
                    key = id(pop.sem)
                    if waited.get(key, 0) >= pop.val:
                        continue
                    cur = need.get(key)
                    if cur is None or cur[1] < pop.val:
                        need[key] = (pop.sem, pop.val)
                for key, (s, v) in need.items():
                    eng.wait_ge(s, v)
                    waited[key] = v
                if op.kind in ("fence", "barrier"):
                    continue
                inst = op.fn(eng)
                if op.signal:
                    inst.then_inc(op.sem, 16 if op.kind == "dma" else 1)

        with nc.Block() as block:
            @block.sync
            def _(e):
                emit("sp", e)

            @block.tensor
            def _(e):
                emit("pe", e)

            @block.scalar
            def _(e):
                emit("act", e)

            @block.vector
            def _(e):
                emit("dve", e)

            @block.gpsimd
            def _(e):
                emit("pool", e)
        self.stack.close()


def _tt(P, eng, out, in0, in1, op, r, w):
    return P._add(eng, lambda e: e.tensor_tensor(out=out, in0=in0, in1=in1, op=op), r, w)


def _ts(P, eng, out, in0, s1, s2, op0, op1, r, w, accum_out=None):
    if accum_out is not None:
        return P._add(eng, lambda e: e.tensor_scalar(out=out, in0=in0, scalar1=s1, scalar2=s2, op0=op0, op1=op1,
                                                     accum_out=accum_out), r, w)
    if op1 is None:
        return P._add(eng, lambda e: e.tensor_scalar(out=out, in0=in0, scalar1=s1, scalar2=None, op0=op0), r, w)
    return P._add(eng, lambda e: e.tensor_scalar(out=out, in0=in0, scalar1=s1, scalar2=s2, op0=op0, op1=op1), r, w)


def _stt(P, eng, out, in0, scalar, in1, op0, op1, r, w):
    return P._add(eng, lambda e: e.scalar_tensor_tensor(out=out, in0=in0, scalar=scalar, in1=in1, op0=op0, op1=op1), r, w)


def _act(P, out, in_, func, r, w, bias=None, scale=None, accum_out=None):
    kw = {}
    if bias is not None:
        kw["bias"] = bias
    if scale is not None:
        kw["scale"] = scale
    if accum_out is not None:
        kw["accum_out"] = accum_out
    return P._add("act", lambda e: e.activation(out=out, in_=in_, func=func, **kw), r, w)


def _cp(P, eng, out, in_, r, w):
    if eng == "act":
        return P._add("act", lambda e: e.activation(out=out, in_=in_, func=AF.Copy), r, w)
    return P._add(eng, lambda e: e.tensor_copy(out=out, in_=in_), r, w)


def _mm(P, out, lhsT, rhs, start, stop, r, w):
    return P._add("pe", lambda e: e.matmul(out, lhsT, rhs, start=start, stop=stop), r, w)


def _tr(P, out, in_, ident, r, w):
    return P._add("pe", lambda e: e.transpose(out=out, in_=in_, identity=ident), r, w)


def _memset(P, eng, ap, val, w):
    return P._add(eng, lambda e: e.memset(ap, val), [], w)


class Arena:
    def __init__(self, P, name, nbytes):
        self.words = nbytes // 4
        self.t = P.sb(name, [128, self.words], F32)
        self.off = 0

    def reset(self):
        self.off = 0

    def alloc(self, shape, dtype, parts=128):
        n = 1
        for s in shape:
            n *= s
        words = n if dtype == F32 else (n + 1) // 2
        words = (words + 7) // 8 * 8
        assert self.off + words <= self.words, ("arena overflow", self.off, words, self.words)
        ap = self.t[0:parts, self.off:self.off + words]
        self.off += words
        if dtype != F32:
            ap = ap.bitcast(dtype)
        ap = ap[:, 0:n]
        if len(shape) == 2:
            ap = ap.rearrange("p (a b) -> p a b", a=shape[0])
        elif len(shape) == 3:
            ap = ap.rearrange("p (a b c) -> p a b c", a=shape[0], b=shape[1])
        elif len(shape) == 4:
            ap = ap.rearrange("p (a b c d) -> p a b c d", a=shape[0], b=shape[1], c=shape[2])
        return ap


D = 1024
KC = 8
NEXP = 16384
EVEN_IN = 3088
ODD_IN = 1536
EPS = 1e-6
GRP = 6
ECH = 512


def build_program(NLB, NCB, DEPTH, only=None):
    nc = bass.Bass("TRN2", target_bir_lowering=False)
    NB = NLB + NCB
    T = NB * 128
    TL = NLB * 128
    NE = (DEPTH + 1) // 2
    NO = max(DEPTH // 2, 1)

    def dram(name, shape, kind="ExternalInput"):
        return nc.dram_tensor(name, list(shape), F32, kind=kind).ap()

    x_d = dram("x", [TL, D])
    ctx_d = dram("ctx", [NCB * 128, D])
    c2_d = dram("c2", [2, D])
    mod_w_d = dram("mod_w", [DEPTH, D, 6 * D])
    mod_b_d = dram("mod_b", [DEPTH, 6 * D])
    n1g_d = dram("norm1_g", [DEPTH, D])
    n2g_d = dram("norm2_g", [DEPTH, D])
    ev_w_in_d = dram("ev_w_in", [NE, D, EVEN_IN])
    ev_w_sw_d = dram("ev_w_sw", [NE, D, 1024])
    conv_w_d = dram("ev_ml_conv_w", [NE, 3, 512])
    conv_b_d = dram("ev_ml_conv_b", [NE, 512])
    gate_b_d = dram("ev_ml_gate_b", [NE, 16])
    ml_g_d = dram("ev_ml_norm_g", [NE, 512])
    da_lam_d = dram("ev_da_lam", [NE, 256])
    da_g_d = dram("ev_da_norm_g", [NE, 512])
    ev_w_out_d = dram("ev_w_out", [NE, D, D])
    od_w_in_d = dram("od_w_in", [NO, D, ODD_IN])
    od_w_sw_d = dram("od_w_sw", [NO, D, 1280])
    od_sink_d = dram("od_sink", [NO, 16])
    od_w_out_d = dram("od_w_out", [NO, D, D])
    pr_w_q_d = dram("pr_w_q", [DEPTH, D, 2048])
    pr_keysT_d = dram("pr_keysT", [DEPTH, 16, 128, 128])
    pr_uT_d = dram("pr_uT", [DEPTH, D, NEXP])
    pr_v_d = dram("pr_v", [DEPTH, NEXP, D])
    final_g_d = dram("final_g", [1, D])
    rope_cos_d = dram("rope_cos", [128, TL])
    rope_sin_d = dram("rope_sin", [128, TL])
    out_d = dram("out", [TL, D], kind="ExternalOutput")
    hbuf_d = dram("hbuf", [T, D], kind="Internal")

    P = Prog(nc)
    identb = P.sb("identb", [128, 128], BF16)
    identf = P.sb("identf", [128, 128], F32)
    tri_le = P.sb("tri_le", [128, 128], BF16)
    tri_ge = P.sb("tri_ge", [128, 128], BF16)
    sel = P.sb("sel", [2, 2, 128], F32)
    xT = P.sb("xT", [128, KC, T], BF16)
    hA = P.sb("hA", [128, D], F32)
    hB = P.sb("hB", [128, D], F32)
    hC = P.sb("hC", [128, D], BF16)
    gates = P.sb("gates", [128, 4, D], F32)
    cols = P.sb("cols", [128, 4, KC, 2], F32)
    small = P.sb("small", [128, 64], F32)
    ps = [P.ps("ps%d" % i, [128, 512], F32) for i in range(8)]
    psb = [p[:].bitcast(BF16) for p in ps]
    AR = Arena(P, "arena", 135 * 1024)
    stg = P.sb("stg", [128, 3, 512], F32)
    stg_n = [0]

    def load_cast(dst, src, wname):
        A_, B_ = dst.shape[1], dst.shape[2]
        if B_ >= 512:
            pieces = [(a, 1, b0, min(512, B_ - b0)) for a in range(A_) for b0 in range(0, B_, 512)]
        else:
            per = max(1, 512 // B_)
            pieces = [(a, min(per, A_ - a), 0, B_) for a in range(0, A_, per)]
        for (a0, an, b0, bn) in pieces:
            k = stg_n[0] % 3
            stg_n[0] += 1
            sv = stg[:, k, 0:an * bn].rearrange("p (a b) -> p a b", a=an)
            P.dma(sv, src[:, a0:a0 + an, b0:b0 + bn], reads=[], writes=[("stg", k)])
            _cp(P, "act", dst[:, a0:a0 + an, b0:b0 + bn], sv, [("stg", k)], [wname])

    def PSr(i):
        return ("ps", i)

    XTALL = [("xT", b_) for b_ in range(NB)]

    _memset(P, "pool", identb[:], 0.0, ["identb"])
    P._add("pool", lambda e: e.affine_select(out=identb[:], in_=identb[:], pattern=[[-1, 128]], compare_op=ALU.not_equal,
                                             fill=1.0, base=0, channel_multiplier=1), ["identb"], ["identb"])
    _memset(P, "pool", identf[:], 0.0, ["identf"])
    P._add("pool", lambda e: e.affine_select(out=identf[:], in_=identf[:], pattern=[[-1, 128]], compare_op=ALU.not_equal,
                                             fill=1.0, base=0, channel_multiplier=1), ["identf"], ["identf"])
    _memset(P, "pool", tri_le[:], 1.0, ["tri_le"])
    P._add("pool", lambda e: e.affine_select(out=tri_le[:], in_=tri_le[:], pattern=[[1, 128]], compare_op=ALU.is_ge,
                                             fill=0.0, base=0, channel_multiplier=-1), ["tri_le"], ["tri_le"])
    _memset(P, "pool", tri_ge[:], 1.0, ["tri_ge"])
    P._add("pool", lambda e: e.affine_select(out=tri_ge[:], in_=tri_ge[:], pattern=[[-1, 128]], compare_op=ALU.is_ge,
                                             fill=0.0, base=0, channel_multiplier=1), ["tri_ge"], ["tri_ge"])
    _memset(P, "pool", sel[:], 1.0, ["sel"])
    P._add("pool", lambda e: e.affine_select(out=sel[:, 0, :], in_=sel[:, 0, :], pattern=[[0, 128]], compare_op=ALU.is_equal,
                                             fill=0.0, base=0, channel_multiplier=1), ["sel"], ["sel"])
    P._add("pool", lambda e: e.affine_select(out=sel[:, 1, :], in_=sel[:, 1, :], pattern=[[0, 128]], compare_op=ALU.is_equal,
                                             fill=0.0, base=-1, channel_multiplier=1), ["sel"], ["sel"])

    hver = [0] * NB

    def h_src(blk):
        if hver[blk] == 0:
            if blk < NLB:
                return x_d[blk * 128:(blk + 1) * 128, :]
            return ctx_d[(blk - NLB) * 128:(blk - NLB + 1) * 128, :]
        return hbuf_d[blk * 128:(blk + 1) * 128, :]

    def load_h(blk, tile, tname):
        P.dma(tile[:], h_src(blk), reads=[("h", blk)], writes=[tname])

    def store_h(blk, tile, tname):
        P.dma(hbuf_d[blk * 128:(blk + 1) * 128, :], tile[:], reads=[tname], writes=[("h", blk)])
        hver[blk] = 1

    def norm_to_xT(blk, tile, tname, which):
        lc = 0 if blk < NLB else 1
        ss = small[:, 0:1]
        rs = small[:, 1:2]
        _act(P, hC[:], tile[:], AF.Square, [tname], ["hC", "ss"], accum_out=ss)
        _ts(P, "dve", rs, ss, 1.0 / D, EPS, ALU.mult, ALU.add, ["ss"], ["rs"])
        _act(P, rs, rs, AF.Sqrt, ["rs"], ["rs"])
        P._add("dve", lambda e: e.reciprocal(out=rs, in_=rs), ["rs"], ["rs"])
        _ts(P, "dve", hC[:], tile[:], rs, None, ALU.mult, None, [tname, "rs", "hC"], ["hC"])
        for kc in range(KC):
            _tr(P, psb[7][:, kc * 128:(kc + 1) * 128], hC[:, kc * 128:(kc + 1) * 128], identb[:], ["hC", "identb"], [PSr(7)])
        for kc in range(KC):
            _ts(P, "dve", xT[:, kc, blk * 128:(blk + 1) * 128], psb[7][:, kc * 128:(kc + 1) * 128],
                cols[:, 2 * which, kc, lc:lc + 1], cols[:, 2 * which + 1, kc, lc:lc + 1], ALU.mult, ALU.add,
                [PSr(7), "cols"], [("xT", blk)])

    def modvec(l):
        AR.reset()
        cT = AR.alloc([KC, 2], F32)
        rows = AR.alloc([6 * D], F32, parts=2)
        wst = AR.alloc([2, KC, 512], F32)
        gcol = AR.alloc([2, KC], F32)
        for r_ in range(2):
            P.dma(cT[:, :, r_], c2_d[r_:r_ + 1, :].rearrange("o (k p) -> p (o k)", p=128), reads=[], writes=["cT"],
                  allow_slow_non_contiguous=True)
        P.dma(gcol[:, 0, :], n1g_d[l:l + 1, :].rearrange("o (k p) -> p (o k)", p=128), reads=[], writes=["gcol"],
              allow_slow_non_contiguous=True)
        P.dma(gcol[:, 1, :], n2g_d[l:l + 1, :].rearrange("o (k p) -> p (o k)", p=128), reads=[], writes=["gcol"],
              allow_slow_non_contiguous=True)
        _act(P, cT, cT, AF.Silu, ["cT"], ["cT"])
        P.dma(rows[0:1, :], mod_b_d[l:l + 1, :], reads=[], writes=["rows"])
        P.dma(rows[1:2, :], mod_b_d[l:l + 1, :], reads=[], writes=["rows"])
        for n in range(12):
            b = n % 2
            P.dma(wst[:, b], mod_w_d[l].rearrange("(k p) n -> p k n", p=128)[:, :, n * 512:(n + 1) * 512],
                  reads=[], writes=[("wst", b)])
            for kc in range(KC):
                _mm(P, ps[0][0:2, :], cT[:, kc, :], wst[:, b, kc, :], kc == 0, kc == KC - 1,
                    ["cT", ("wst", b)], [PSr(0)])
            _tt(P, "dve", rows[0:2, n * 512:(n + 1) * 512], ps[0][0:2, :], rows[0:2, n * 512:(n + 1) * 512], ALU.add,
                [PSr(0), "rows"], ["rows"])
        for gi, idx in enumerate((2, 5)):
            for lc in range(2):
                for half in range(2):
                    _mm(P, ps[1][:, :], sel[:, lc, :], rows[0:2, idx * D + half * 512: idx * D + (half + 1) * 512],
                        True, True, ["sel", "rows"], [PSr(1)])
                    _cp(P, "act", gates[:, gi * 2 + lc, half * 512:(half + 1) * 512], ps[1][:, :], [PSr(1)], ["gates"])
        for wi, (sh, sc) in enumerate(((0, 1), (3, 4))):
            for kc in range(KC):
                _tr(P, ps[2][:, kc * 4:kc * 4 + 2], rows[0:2, sc * D + kc * 128: sc * D + (kc + 1) * 128], identf[0:2, 0:2],
                    ["rows", "identf"], [PSr(2)])
                _tr(P, ps[2][:, kc * 4 + 2:kc * 4 + 4], rows[0:2, sh * D + kc * 128: sh * D + (kc + 1) * 128], identf[0:2, 0:2],
                    ["rows", "identf"], [PSr(2)])
            for kc in range(KC):
                for lc in range(2):
                    _stt(P, "dve", cols[:, 2 * wi, kc, lc:lc + 1], ps[2][:, kc * 4 + lc:kc * 4 + lc + 1], 1.0,
                         gcol[:, wi, kc:kc + 1], ALU.add, ALU.mult, [PSr(2), "gcol"], ["cols"])
                _cp(P, "dve", cols[:, 2 * wi + 1, kc, :], ps[2][:, kc * 4 + 2:kc * 4 + 4], [PSr(2)], ["cols"])
        P.barrier()

    def peer(l, blocks, after_block):
        AR.reset()
        keysT = AR.alloc([16, 128], BF16)
        load_cast(keysT, pr_keysT_d[l].rearrange("f d k -> d f k"), "keysT")
        wq = AR.alloc([1, KC, 128], BF16)
        qTs = AR.alloc([GRP * 128], BF16)
        sst = AR.alloc([GRP, 16, 128], F32)
        acc = AR.alloc([GRP, D], F32)
        uT = AR.alloc([2, KC, ECH], BF16)
        vch = AR.alloc([2, ECH // 128, D], BF16)
        v16 = AR.alloc([16, 16], F32)
        tmp128 = AR.alloc([128], F32)
        cand = AR.alloc([256], F32)
        cand2 = AR.alloc([256], F32)
        cand3 = AR.alloc([256], F32)
        ctop = AR.alloc([8, 24], F32)
        ez = AR.alloc([8, 16], F32)
        st = AR.alloc([GRP, 8, 4], F32)
        nb1 = AR.alloc([8], F32)
        nb2 = AR.alloc([8], F32)
        zz = AR.alloc([8], F32)
        gA = AR.alloc([2, ECH], F32)
        ex = AR.alloc([3, ECH], F32)
        gg = AR.alloc([3, ECH], BF16)
        Wb = AR.alloc([ECH], BF16)
        WT = AR.alloc([ECH // 128, 128], BF16)
        NCH = NEXP // ECH
        IPC = ECH // 128
        groups = [blocks[i:i + GRP] for i in range(0, len(blocks), GRP)]
        for grp in groups:
            ng = len(grp)
            for fc in range(16):
                b = 0
                load_cast(wq[:, b], pr_w_q_d[l].rearrange("(k p) n -> p k n", p=128)[:, :, fc * 128:(fc + 1) * 128], ("wq", b))
                for gi, blk in enumerate(grp):
                    for kc in range(KC):
                        _mm(P, ps[0][:, gi * 128 % 512:gi * 128 % 512 + 128] if gi < 4 else ps[1][:, (gi - 4) * 128:(gi - 3) * 128],
                            wq[:, b, kc, :], xT[:, kc, blk * 128:(blk + 1) * 128], kc == 0, kc == KC - 1,
                            [("wq", b), ("xT", blk)], [PSr(0 if gi < 4 else 1)])
                n0 = min(ng, 4)
                _cp(P, "act", qTs[:, 0:n0 * 128], ps[0][:, 0:n0 * 128], [PSr(0)], ["qTs"])
                if ng > 4:
                    _cp(P, "act", qTs[:, 512:ng * 128], ps[1][:, 0:(ng - 4) * 128], [PSr(1)], ["qTs"])
                for gi, blk in enumerate(grp):
                    pb = 2 + (gi // 4)
                    _mm(P, ps[pb][:, (gi % 4) * 128:(gi % 4 + 1) * 128], qTs[:, gi * 128:(gi + 1) * 128], keysT[:, fc, :],
                        True, True, ["qTs", "keysT"], [PSr(pb)])
                _cp(P, "dve", sst[:, 0:n0, fc, :], ps[2][:, 0:n0 * 128].rearrange("p (g k) -> p g k", g=n0), [PSr(2)], ["sst"])
                if ng > 4:
                    _cp(P, "dve", sst[:, 4:ng, fc, :], ps[3][:, 0:(ng - 4) * 128].rearrange("p (g k) -> p g k", g=ng - 4),
                        [PSr(3)], ["sst"])
            for gi, blk in enumerate(grp):
                for fc in range(16):
                    P._add("dve", lambda e, fc=fc, gi=gi: e.max(out=v16[:, fc, 0:8], in_=sst[:, gi, fc, :]), ["sst"], ["v16"])
                    P._add("dve", lambda e, fc=fc, gi=gi: e.match_replace(out=tmp128, in_to_replace=v16[:, fc, 0:8],
                                                                         in_values=sst[:, gi, fc, :], imm_value=-1e30),
                           ["sst", "v16"], ["tmp128"])
                    P._add("dve", lambda e, fc=fc: e.max(out=v16[:, fc, 8:16], in_=tmp128), ["tmp128"], ["v16"])
                v4 = v16.rearrange("p (h t) k -> p h t k", t=2)
                for h in range(8):
                    _tt(P, "dve", cand.rearrange("p (a b) -> p a b", a=16),
                        v4[:, h, 0, :].unsqueeze(2).to_broadcast([128, 16, 16]),
                        v4[:, h, 1, :].unsqueeze(1).to_broadcast([128, 16, 16]), ALU.add, ["v16"], ["cand"])
                    P._add("dve", lambda e, h=h: e.max(out=ctop[:, h, 0:8], in_=cand), ["cand"], ["ctop"])
                    P._add("dve", lambda e, h=h: e.match_replace(out=cand2, in_to_replace=ctop[:, h, 0:8], in_values=cand,
                                                                 imm_value=-1e30), ["cand", "ctop"], ["cand2"])
                    P._add("dve", lambda e, h=h: e.max(out=ctop[:, h, 8:16], in_=cand2), ["cand2"], ["ctop"])
                    P._add("dve", lambda e, h=h: e.match_replace(out=cand3, in_to_replace=ctop[:, h, 8:16], in_values=cand2,
                                                                 imm_value=-1e30), ["cand2", "ctop"], ["cand3"])
                    P._add("dve", lambda e, h=h: e.max(out=ctop[:, h, 16:24], in_=cand3), ["cand3"], ["ctop"])
                _tt(P, "dve", ez, ctop[:, :, 0:16], ctop[:, :, 0:1].to_broadcast([128, 8, 16]), ALU.subtract, ["ctop"], ["ez"])
                _act(P, ez, ez, AF.Exp, ["ez"], ["ez"])
                P._add("dve", lambda e: e.tensor_reduce(out=zz, in_=ez, axis=AX.X, op=ALU.add), ["ez"], ["zz"])
                _act(P, zz, zz, AF.Ln, ["zz"], ["zz"])
                _ts(P, "dve", nb1, v4[:, :, 0, 0], -1.0, None, ALU.mult, None, ["v16"], ["nb1"])
                _stt(P, "dve", nb2, v4[:, :, 1, 0], -1.0, zz, ALU.mult, ALU.subtract, ["v16", "zz"], ["nb2"])
                thr = st[:, gi, :, 1]
                _tt(P, "dve", thr, ctop[:, :, 15], ctop[:, :, 16], ALU.add, ["ctop"], ["st"])
                _stt(P, "dve", thr, thr, 0.5, ctop[:, :, 0], ALU.mult, ALU.subtract, ["st", "ctop"], ["st"])
                _tt(P, "dve", thr, thr, zz, ALU.subtract, ["st", "zz"], ["st"])
                _act(P, st[:, gi, :, 0], thr, AF.Exp, ["st"], ["st"])
                for h in range(8):
                    _act(P, sst[:, gi, 2 * h, :], sst[:, gi, 2 * h, :], AF.Exp, ["sst", "nb1"], ["sst"], bias=nb1[:, h:h + 1])
                    _act(P, sst[:, gi, 2 * h + 1, :], sst[:, gi, 2 * h + 1, :], AF.Exp, ["sst", "nb2"], ["sst"],
                         bias=nb2[:, h:h + 1])
            def load_chunk(c):
                b = c % 2
                load_cast(uT[:, b], pr_uT_d[l].rearrange("(k p) e -> p k e", p=128)[:, :, c * ECH:(c + 1) * ECH], ("uT", b))
                load_cast(vch[:, b], pr_v_d[l][c * ECH:(c + 1) * ECH, :].rearrange("(s p) d -> p s d", p=128), ("vch", b))
            load_chunk(0)
            nx = [0]
            ACT_HEADS = ()

            def stage1(k, c, gi, blk, b):
                ga = k % 2
                pg = 3 if k % 2 == 0 else 6
                for kc in range(KC):
                    _mm(P, ps[0][:, :], xT[:, kc, blk * 128:(blk + 1) * 128], uT[:, b, kc, :], kc == 0, kc == KC - 1,
                        [("xT", blk), ("uT", b)], [PSr(0)])
                _act(P, gA[:, ga], ps[0][:, :], AF.Gelu, [PSr(0)], [("gA", ga)])
                for h in range(8):
                    eb = nx[0] % 3
                    nx[0] += 1
                    e2 = sst[:, gi, 2 * h + 1, :]
                    if h in ACT_HEADS:
                        for i_ in range(IPC):
                            _act(P, ex[:, eb, i_ * 128:(i_ + 1) * 128], e2, AF.Copy, ["sst"], [("ex", eb)],
                                 scale=sst[:, gi, 2 * h, c * IPC + i_:c * IPC + i_ + 1])
                    else:
                        e1 = sst[:, gi, 2 * h, c * IPC:(c + 1) * IPC]
                        _tt(P, "dve", ex[:, eb].rearrange("p (i j) -> p i j", i=IPC),
                            e1.unsqueeze(2).to_broadcast([128, IPC, 128]), e2.unsqueeze(1).to_broadcast([128, IPC, 128]),
                            ALU.mult, ["sst"], [("ex", eb)])
                    _stt(P, "dve", gg[:, eb], ex[:, eb], st[:, gi, h, 0:1], ex[:, eb], ALU.is_ge, ALU.mult,
                         [("ex", eb), "st"], [("gg", eb)])
                    _mm(P, ps[pg][:, :], identb[:], gg[:, eb], h == 0, h == 7, [("gg", eb), "identb"], [PSr(pg)])

            def stage2(k, c, gi, blk, b):
                ga = k % 2
                pg = 3 if k % 2 == 0 else 6
                pp = (4, 5) if k % 2 == 0 else (1, 7)
                _tt(P, "dve", Wb, ps[pg][:, :], gA[:, ga], ALU.mult, [PSr(pg), ("gA", ga)], ["Wb"])
                for s_ in range(ECH // 128):
                    _tr(P, psb[2][:, s_ * 128:(s_ + 1) * 128], Wb[:, s_ * 128:(s_ + 1) * 128], identb[:],
                        ["Wb", "identb"], [PSr(2)])
                _cp(P, "act", WT, psb[2][:, 0:ECH].rearrange("p (s t) -> p s t", s=ECH // 128), [PSr(2)], ["WT"])
                for half in range(2):
                    for s_ in range(ECH // 128):
                        _mm(P, ps[pp[half]][:, :], WT[:, s_, :], vch[:, b, s_, half * 512:(half + 1) * 512],
                            s_ == 0, s_ == ECH // 128 - 1, ["WT", ("vch", b)], [PSr(pp[half])])

            def stage3(k, c, gi, blk, b):
                pp = (4, 5) if k % 2 == 0 else (1, 7)
                for half in range(2):
                    if c == 0:
                        _cp(P, "dve", acc[:, gi, half * 512:(half + 1) * 512], ps[pp[half]][:, :], [PSr(pp[half])],
                            [("acc", gi)])
                    else:
                        _tt(P, "dve", acc[:, gi, half * 512:(half + 1) * 512], ps[pp[half]][:, :],
                            acc[:, gi, half * 512:(half + 1) * 512], ALU.add, [PSr(pp[half]), ("acc", gi)], [("acc", gi)])

            pend2 = None
            pend3 = None
            k_ = 0
            for c in range(NCH):
                b = c % 2
                for gi, blk in enumerate(grp):
                    cur = (k_, c, gi, blk, b)
                    stage1(*cur)
                    if pend2 is not None:
                        stage2(*pend2)
                    if pend3 is not None:
                        stage3(*pend3)
                    pend3 = pend2
                    pend2 = cur
                    k_ += 1
                    if gi == min(1, ng - 1) and c + 1 < NCH:
                        load_chunk(c + 1)
            if pend2 is not None:
                stage2(*pend2)
            if pend3 is not None:
                stage3(*pend3)
            if pend2 is not None:
                stage3(*pend2)
            for gi, blk in enumerate(grp):
                lc = 0 if blk < NLB else 1
                load_h(blk, hA, "hA")
                _tt(P, "dve", acc[:, gi, :], acc[:, gi, :], gates[:, 2 + lc, :], ALU.mult, [("acc", gi), "gates"], [("acc", gi)])
                _tt(P, "dve", hA[:], hA[:], acc[:, gi, :], ALU.add, ["hA", ("acc", gi)], ["hA"])
                after_block(blk, hA, "hA")
        P.barrier()

    class _Pipe:
        def __init__(self):
            self.pending = None
            self.posts = []

        def _flush(self):
            if self.pending is not None:
                self.pending()
                self.pending = None
            for f in self.posts:
                f()
            self.posts = []

        def step(self, a_fn, b_fn):
            a_fn()
            self._flush()
            self.pending = b_fn

        def post(self, fn):
            self.posts.append(fn)

        def flush(self):
            self._flush()

    PIPE = _Pipe()

    def attend_step(qT_ap, kT_ap, vaug_ap, out_ps, out_res, first, last, func, scale, mask, r_q, r_k, r_v, sbank, Et, Ename):
        def a_fn():
            _mm(P, ps[sbank][:, 0:128], kT_ap, qT_ap, True, True, [r_q, r_k], [PSr(sbank)])
            _act(P, Et, ps[sbank][:, 0:128], func, [PSr(sbank)] + ([scale[1]] if isinstance(scale, tuple) else []), [Ename],
                 scale=(scale[0] if isinstance(scale, tuple) else scale))
            if mask is not None:
                _tt(P, "pool", Et, Et, mask[:], ALU.mult, [Ename], [Ename])

        def b_fn():
            _mm(P, out_ps, Et, vaug_ap, first, last, [Ename, r_v], [out_res])
        PIPE.step(a_fn, b_fn)

    def odd_mixer(l, j, need_ctx, after_block):
        AR.reset()
        cosT = AR.alloc([TL], F32)
        sinT = AR.alloc([TL], F32)
        P.dma(cosT, rope_cos_d, reads=[], writes=["cosT"])
        P.dma(sinT, rope_sin_d, reads=[], writes=["sinT"])
        qT = AR.alloc([8, T], BF16)
        kT = AR.alloc([4, T], BF16)
        vaug = AR.alloc([NB, 4, 65], BF16)
        wst = AR.alloc([2, KC, 512], BF16)
        tmp1 = AR.alloc([512], F32)
        tmp2 = AR.alloc([512], F32)
        esink = AR.alloc([16], F32)
        Et = AR.alloc([4, 128], BF16)
        att = AR.alloc([D], BF16)
        attT = AR.alloc([KC, 128], BF16)
        rden = AR.alloc([4], F32)
        AR_attT[0] = attT
        P.dma(esink, od_sink_d[j:j + 1, :].to_broadcast([128, 16]), reads=[], writes=["esink"])
        _act(P, esink, esink, AF.Exp, ["esink"], ["esink"])
        _memset(P, "pool", vaug[:, :, :, 64:65], 1.0, ["vaug"])
        wv = od_w_in_d[j].rearrange("(k p) n -> p k n", p=128)
        wsw = od_w_sw_d[j].rearrange("(k p) n -> p k n", p=128)
        TG = [(t0, min(512, T - t0)) for t0 in range(0, T, 512)]
        nld = [0]

        def loadw(src, c0, ncol):
            b = nld[0] % 2
            nld[0] += 1
            load_cast(wst[:, b, :, 0:ncol], src[:, :, c0:c0 + ncol], ("wst", b))
            return b

        for ci in range(12):
            dst = qT[:, ci, :] if ci < 8 else kT[:, ci - 8, :]
            dname = ("qT", ci) if ci < 8 else ("kT", ci - 8)
            if ci % 4 == 0:
                ncol = 512 if ci < 8 else 256
                b0 = loadw(wv, ci * 128 if ci < 8 else 1024, ncol)
                b1 = loadw(wsw, ci * 128 if ci < 8 else 1024, ncol)
            if ci < 8:
                parts = [(0, 128, (ci % 4) * 128, 128)]
            else:
                parts = [(0, 64, (ci - 8) * 64, 64), (64, 128, (ci - 8) * 64, 64)]
            for (t0, tn) in TG:
                tl = max(0, min(tn, TL - t0))
                for (p0, p1, cc, cn) in parts:
                    for kc in range(KC):
                        _mm(P, ps[0][p0:p1, 0:tn], wst[:, b0, kc, cc:cc + cn], xT[:, kc, t0:t0 + tn], kc == 0, kc == KC - 1,
                            [("wst", b0)] + XTALL, [PSr(0)])
                    if tl > 0:
                        for kc in range(KC):
                            _mm(P, ps[1][p0:p1, 0:tl], wst[:, b1, kc, cc:cc + cn], xT[:, kc, t0:t0 + tl], kc == 0, kc == KC - 1,
                                [("wst", b1)] + XTALL, [PSr(1)])
                if tl > 0:
                    _tt(P, "dve", tmp1[:, 0:tl], ps[0][:, 0:tl], cosT[:, t0:t0 + tl], ALU.mult, [PSr(0), "cosT"], ["tmp1"])
                    _tt(P, "dve", tmp2[:, 0:tl], ps[1][:, 0:tl], sinT[:, t0:t0 + tl], ALU.mult, [PSr(1), "sinT"], ["tmp2"])
                    _tt(P, "dve", dst[:, t0:t0 + tl], tmp1[:, 0:tl], tmp2[:, 0:tl], ALU.add, ["tmp1", "tmp2"], [dname])
                if tl < tn:
                    _cp(P, "act", dst[:, t0 + tl:t0 + tn], ps[0][:, tl:tn], [PSr(0)], [dname])
        bv = loadw(wv, 1280, 256)
        for blk in range(NB):
            for kc in range(KC):
                _mm(P, ps[2][:, 0:256], xT[:, kc, blk * 128:(blk + 1) * 128], wst[:, bv, kc, 0:256], kc == 0, kc == KC - 1,
                    [("wst", bv)] + XTALL, [PSr(2)])
            _cp(P, "act", vaug[:, blk, :, 0:64], ps[2][:, 0:256].rearrange("p (h d) -> p h d", h=4), [PSr(2)], ["vaug"])
        wo = AR.alloc([KC, D], BF16)
        load_cast(wo, od_w_out_d[j].rearrange("(k p) n -> p k n", p=128), "wo")
        qblocks = list(range(NLB)) + (list(range(NLB, NB)) if need_ctx else [])
        nE = [0]
        for qb in qblocks:
            if qb < NLB:
                keys = [(kb, None) for kb in range(NLB, NB)]
                if qb > 0:
                    keys.append((qb - 1, tri_ge))
                keys.append((qb, None))
                if qb + 1 < NLB:
                    keys.append((qb + 1, tri_le))
            else:
                keys = [(kb, None) for kb in range(NLB, NB)]
            for g in range(4):
                for hh in range(4):
                    hq = g * 4 + hh
                    qa = qT[(hq % 2) * 64:(hq % 2) * 64 + 64, hq // 2, qb * 128:(qb + 1) * 128]
                    for ki, (kb, mask) in enumerate(keys):
                        ka = kT[(hq % 2) * 64:(hq % 2) * 64 + 64, g, kb * 128:(kb + 1) * 128]
                        eb = nE[0] % 4
                        sb_ = 1 + nE[0] % 3
                        nE[0] += 1
                        attend_step(qa, ka, vaug[:, kb, g, :], ps[4 + g % 2][:, hh * 65:hh * 65 + 65], PSr(4 + g % 2),
                                    ki == 0, ki == len(keys) - 1, AF.Exp, 0.125, mask,
                                    ("qT", hq // 2), ("kT", g), "vaug", sb_, Et[:, eb, :], ("Et", eb))
                def post_odd(g=g):
                    po = ps[4 + g % 2][:, 0:260].rearrange("p (h d) -> p h d", h=4)
                    _tt(P, "dve", rden, po[:, :, 64], esink[:, g * 4:(g + 1) * 4], ALU.add, [PSr(4 + g % 2), "esink"], ["rden"])
                    P._add("dve", lambda e: e.reciprocal(out=rden, in_=rden), ["rden"], ["rden"])
                    _tt(P, "dve", att[:, g * 256:(g + 1) * 256].rearrange("p (h d) -> p h d", h=4), po[:, :, 0:64],
                        rden.unsqueeze(2).to_broadcast([128, 4, 64]), ALU.mult, [PSr(4 + g % 2), "rden"], ["att"])
                PIPE.post(post_odd)
            PIPE.post(lambda qb=qb: out_proj_residual(qb, att, wo, after_block))
        PIPE.flush()
        P.barrier()

    def out_proj_residual(qb, att, wo, after_block):
        attT = AR_attT[0]
        lc = 0 if qb < NLB else 1
        for kc in range(KC):
            _tr(P, psb[6][:, kc * 128:(kc + 1) * 128], att[:, kc * 128:(kc + 1) * 128], identb[:], ["att", "identb"], [PSr(6)])
        _cp(P, "act", attT, psb[6][:, 0:D].rearrange("p (k t) -> p k t", k=KC), [PSr(6)], ["attT"])
        load_h(qb, hA, "hA")
        for half in range(2):
            for kc in range(KC):
                _mm(P, ps[0][:, :], attT[:, kc, :], wo[:, kc, half * 512:(half + 1) * 512], kc == 0, kc == KC - 1,
                    ["attT", "wo"], [PSr(0)])
            _tt(P, "dve", hB[:, half * 512:(half + 1) * 512], ps[0][:, :], gates[:, lc, half * 512:(half + 1) * 512], ALU.mult,
                [PSr(0), "gates"], ["hB"])
            _tt(P, "dve", hA[:, half * 512:(half + 1) * 512], hA[:, half * 512:(half + 1) * 512],
                hB[:, half * 512:(half + 1) * 512], ALU.add, ["hA", "hB"], ["hA"])
        after_block(qb, hA, "hA")

    AR_attT = [None]


    def even_mixer(l, j, need_ctx, after_block, lam_init):
        AR.reset()
        wv = ev_w_in_d[j].rearrange("(k p) n -> p k n", p=128)
        wsw = ev_w_sw_d[j].rearrange("(k p) n -> p k n", p=128)
        TG = [(t0, min(512, T - t0)) for t0 in range(0, T, 512)]
        wst = AR.alloc([2, KC, 512], BF16)
        nld = [0]

        def loadw(src, c0, ncol):
            b = nld[0] % 2
            nld[0] += 1
            load_cast(wst[:, b, :, 0:ncol], src[:, :, c0:c0 + ncol], ("wst", b))
            return b
        og = AR.alloc([NB, 512], BF16)
        mlout = og
        att = AR.alloc([D], BF16)
        attT = AR.alloc([KC, 128], BF16)
        AR_attT[0] = attT
        Et = AR.alloc([4, 128], BF16)
        mlg = AR.alloc([512], F32)
        dag = AR.alloc([512], F32)
        P.dma(mlg, ml_g_d[j:j + 1, :].to_broadcast([128, 512]), reads=[], writes=["mlg"])
        P.dma(dag, da_g_d[j:j + 1, :].to_broadcast([128, 512]), reads=[], writes=["dag"])
        hsum = AR.alloc([512], F32)
        sq = AR.alloc([512], F32)
        s4 = AR.alloc([4], F32)
        s1c = AR.alloc([8], F32)
        mark = AR.off
        qk = AR.alloc([4, T], BF16)
        vml = AR.alloc([NB, 4, 129], BF16)
        GT = AR.alloc([NB, 16], F32)
        PFT = AR.alloc([NB, 16], F32)
        PBT = AR.alloc([NB, 16], F32)
        prefF = AR.alloc([NB, 16], F32)
        prefB = AR.alloc([NB, 16], F32)
        imP = AR.alloc([2, 4, NB], F32)
        KF = AR.alloc([8, NB, NB], F32)
        afac = AR.alloc([2, 4, NB], F32)
        cw = AR.alloc([4, 3], F32)
        cb = AR.alloc([4], F32)
        gb = AR.alloc([1], F32, parts=16)
        tot = AR.alloc([1], F32, parts=16)
        mark2 = AR.off
        bufA = AR.alloc([T], F32, parts=16)
        bufB = AR.alloc([T], F32, parts=16)
        for t_ in range(3):
            P.dma(cw[:, :, t_], conv_w_d[j][t_:t_ + 1, :].rearrange("o (c p) -> p (o c)", p=128), reads=[], writes=["cw"],
                  allow_slow_non_contiguous=True)
        P.dma(cb, conv_b_d[j:j + 1, :].rearrange("o (c p) -> p (o c)", p=128), reads=[], writes=["cb"], allow_slow_non_contiguous=True)
        P.dma(gb, gate_b_d[j:j + 1, :].rearrange("o g -> g o"), reads=[], writes=["gb"], allow_slow_non_contiguous=True)
        _memset(P, "pool", vml[:, :, :, 128:129], 1.0, ["vml"])
        bg = loadw(wv, 1536, 16)
        for (t0, tn) in TG:
            for kc in range(KC):
                _mm(P, ps[3][0:16, 0:tn], wst[:, bg, kc, 0:16], xT[:, kc, t0:t0 + tn], kc == 0, kc == KC - 1,
                    [("wst", bg)] + XTALL, [PSr(3)])
            _ts(P, "dve", bufA[:, t0:t0 + tn], ps[3][0:16, 0:tn], gb[:, 0:1], None, ALU.add, None, [PSr(3), "gb"], ["bufA"])

        def rows_to_tok(src, sname, dstT, dname):
            for blk in range(NB):
                _tr(P, ps[3][:, blk * 16:(blk + 1) * 16], src[:, blk * 128:(blk + 1) * 128], identf[0:16, 0:16],
                    [sname, "identf"], [PSr(3)])
            _cp(P, "dve", dstT, ps[3][:, 0:NB * 16].rearrange("p (b g) -> p b g", b=NB), [PSr(3)], [dname])
        rows_to_tok(bufA, "bufA", GT, "GT")
        _act(P, bufA, bufA, AF.Exp, ["bufA"], ["bufA"], scale=-1.0)
        _act(P, bufA, bufA, AF.Ln, ["bufA"], ["bufA"], bias=1.0)
        _ts(P, "dve", bufA, bufA, -0.5, None, ALU.mult, None, ["bufA"], ["bufA"])
        P._add("dve", lambda e: e.tensor_tensor_scan(out=bufB[:, TL:T], data0=bufA[:, TL:T], data1=bufA[:, TL:T], initial=0.0,
                                                     op0=ALU.add, op1=ALU.add), ["bufA"], ["bufB"])
        _cp(P, "dve", tot, bufB[:, T - 1:T], ["bufB"], ["tot"])
        P._add("dve", lambda e: e.tensor_tensor_scan(out=bufB[:, 0:TL], data0=bufA[:, 0:TL], data1=bufA[:, 0:TL],
                                                     initial=tot[:, 0:1], op0=ALU.add, op1=ALU.add),
               ["bufA", "bufB", "tot"], ["bufB"])
        rows_to_tok(bufB, "bufB", PFT, "PFT")
        P._add("dve", lambda e: e.tensor_tensor_scan(out=bufB[:, 0:T], data0=bufA[:, 0:T], data1=bufA[:, 0:T], initial=0.0,
                                                     op0=ALU.add, op1=ALU.add), ["bufA", "bufB"], ["bufB"])
        _cp(P, "dve", tot, bufB[:, T - 1:T], ["bufB"], ["tot"])
        _stt(P, "dve", bufB, bufB, -1.0, bufA, ALU.mult, ALU.add, ["bufB", "bufA"], ["bufB"])
        _stt(P, "dve", bufB, bufB, tot[:, 0:1], bufA, ALU.add, ALU.add, ["bufB", "bufA", "tot"], ["bufB"])
        rows_to_tok(bufB, "bufB", PBT, "PBT")
        for (src, sname, dst, dname) in ((PFT, "PFT", prefF, "prefF"), (PBT, "PBT", prefB, "prefB")):
            _mm(P, ps[3][:, 0:NB * 16], sel[:, 0, :], src[0:2].rearrange("p b g -> p (b g)"), True, True, ["sel", sname], [PSr(3)])
            _cp(P, "dve", dst, ps[3][:, 0:NB * 16].rearrange("p (b g) -> p b g", b=NB), [PSr(3)], [dname])
        for d_ in range(2):
            PT_ = PFT if d_ == 0 else PBT
            pr_ = prefF if d_ == 0 else prefB
            for h in range(4):
                col = d_ * 8 + 4 + h
                _tt(P, "dve", imP[:, d_, h, :], GT[:, :, d_ * 8 + h], PT_[:, :, col], ALU.subtract,
                    ["GT", "PFT", "PBT"], ["imP"])
                for kb in range(NB):
                    _act(P, KF[:, d_ * 4 + h, kb, :], pr_[:, :, col], AF.Exp, ["prefF", "prefB", "imP"], ["KF"],
                         bias=imP[:, d_, h, kb:kb + 1])
                _tt(P, "dve", afac[:, d_, h, :], PT_[:, :, col], pr_[:, :, col], ALU.subtract,
                    ["PFT", "PBT", "prefF", "prefB"], ["afac"])
        _act(P, afac, afac, AF.Exp, ["afac"], ["afac"])
        _ts(P, "dve", afac, afac, 0.125, None, ALU.mult, None, ["afac"], ["afac"])
        P.barrier()
        AR.off = mark2
        pre = AR.alloc([T], F32)
        cacc = AR.alloc([T], F32)
        bq = loadw(wv, 0, 512)
        for c in range(4):
            for (t0, tn) in TG:
                for kc in range(KC):
                    _mm(P, ps[0][:, 0:tn], wst[:, bq, kc, c * 128:(c + 1) * 128], xT[:, kc, t0:t0 + tn], kc == 0, kc == KC - 1,
                        [("wst", bq)] + XTALL, [PSr(0)])
                _cp(P, "act", pre[:, t0:t0 + tn], ps[0][:, 0:tn], [PSr(0)], ["pre"])
            _ts(P, "dve", cacc, pre, cw[:, c, 1:2], cb[:, c:c + 1], ALU.mult, ALU.add, ["pre", "cw", "cb"], ["cacc"])
            for (a0, a1) in ((0, TL), (TL, T)):
                _stt(P, "dve", cacc[:, a0 + 1:a1], pre[:, a0:a1 - 1], cw[:, c, 0:1], cacc[:, a0 + 1:a1], ALU.mult, ALU.add,
                     ["pre", "cw", "cacc"], ["cacc"])
                _stt(P, "dve", cacc[:, a0:a1 - 1], pre[:, a0 + 1:a1], cw[:, c, 2:3], cacc[:, a0:a1 - 1], ALU.mult, ALU.add,
                     ["pre", "cw", "cacc"], ["cacc"])
            _act(P, qk[:, c, :], cacc, AF.Silu, ["cacc"], [("qk", c)])
        bvv = loadw(wv, 512, 512)
        bo = loadw(wv, 1024, 512)
        for blk in range(NB):
            for kc in range(KC):
                _mm(P, ps[1][:, :], xT[:, kc, blk * 128:(blk + 1) * 128], wst[:, bvv, kc, :], kc == 0, kc == KC - 1,
                    [("wst", bvv)] + XTALL, [PSr(1)])
            _cp(P, "act", vml[:, blk, :, 0:128], ps[1][:, :].rearrange("p (h d) -> p h d", h=4), [PSr(1)], ["vml"])
            for kc in range(KC):
                _mm(P, ps[2][:, :], xT[:, kc, blk * 128:(blk + 1) * 128], wst[:, bo, kc, :], kc == 0, kc == KC - 1,
                    [("wst", bo)] + XTALL, [PSr(2)])
            _act(P, og[:, blk, :], ps[2][:, :], AF.Sigmoid, [PSr(2)], ["og"])
        qblocks = list(range(NLB)) + (list(range(NLB, NB)) if need_ctx else [])
        nE = [0]
        for qb in qblocks:
            for h in range(4):
                qa = qk[(h % 2) * 64:(h % 2) * 64 + 64, h // 2, qb * 128:(qb + 1) * 128]
                for d_ in range(2):
                    if qb < NLB:
                        keys = [(kb, None) for kb in range(NLB, NB)]
                        keys += [(kb, None) for kb in (range(0, qb) if d_ == 0 else range(qb + 1, NLB))]
                    else:
                        keys = [(kb, None) for kb in (range(NLB, qb) if d_ == 0 else range(qb + 1, NB))]
                    keys.append((qb, tri_le if d_ == 0 else tri_ge))
                    pso = 4 + d_
                    for ki, (kb, mask) in enumerate(keys):
                        ka = qk[(h % 2) * 64:(h % 2) * 64 + 64, 2 + h // 2, kb * 128:(kb + 1) * 128]
                        eb = nE[0] % 4
                        sb_ = 1 + nE[0] % 3
                        nE[0] += 1
                        attend_step(qa, ka, vml[:, kb, h, :], ps[pso][:, 0:129], PSr(pso), ki == 0, ki == len(keys) - 1,
                                    AF.Identity, (KF[:, d_ * 4 + h, kb, qb:qb + 1], "KF"), mask,
                                    ("qk", h // 2), ("qk", 2 + h // 2), "vml", sb_, Et[:, eb, :], ("Et", eb))
                    def post_dir(d_=d_, h=h, qb=qb, pso=pso):
                        a_ = afac[:, d_, h, qb:qb + 1]
                        _tt(P, "dve", s1c[:, d_:d_ + 1], ps[pso][:, 128:129], a_, ALU.mult, [PSr(pso), "afac"], ["s1c"])
                        _stt(P, "dve", s1c[:, 2 + d_:3 + d_], s1c[:, d_:d_ + 1], -1.0, s1c[:, d_:d_ + 1], ALU.mult, ALU.max,
                             ["s1c"], ["s1c"])
                        _ts(P, "dve", s1c[:, d_:d_ + 1], s1c[:, 2 + d_:3 + d_], 1.0, None, ALU.max, None, ["s1c"], ["s1c"])
                        P._add("dve", lambda e: e.reciprocal(out=s1c[:, d_:d_ + 1], in_=s1c[:, d_:d_ + 1]), ["s1c"], ["s1c"])
                        _tt(P, "dve", s1c[:, d_:d_ + 1], s1c[:, d_:d_ + 1], a_, ALU.mult, ["s1c", "afac"], ["s1c"])
                        if d_ == 0:
                            _ts(P, "dve", hsum[:, h * 128:(h + 1) * 128], ps[4][:, 0:128], s1c[:, 0:1], None, ALU.mult, None,
                                [PSr(4), "s1c"], ["hsum"])
                        else:
                            _stt(P, "dve", hsum[:, h * 128:(h + 1) * 128], ps[5][:, 0:128], s1c[:, 1:2],
                                 hsum[:, h * 128:(h + 1) * 128], ALU.mult, ALU.add, [PSr(5), "s1c", "hsum"], ["hsum"])
                    PIPE.post(post_dir)
            PIPE.post(lambda qb=qb: headnorm(hsum, sq, s4, mlg, 1.0, mlout[:, qb, :], "og", og[:, qb, :]))
        PIPE.flush()
        P.barrier()
        AR.off = mark
        qda = AR.alloc([4, T], BF16)
        kda = AR.alloc([4, T], BF16)
        vda = AR.alloc([NB, 4, 129], BF16)
        lamt = AR.alloc([256], F32)
        lam = AR.alloc([4], F32)
        r2 = AR.alloc([2], F32)
        o1 = AR.alloc([128], F32)
        mark3 = AR.off
        cosT = AR.alloc([TL], F32)
        sinT = AR.alloc([TL], F32)
        tmp1 = AR.alloc([512], F32)
        tmp2 = AR.alloc([512], F32)
        P.dma(cosT, rope_cos_d, reads=[], writes=["cosT"])
        P.dma(sinT, rope_sin_d, reads=[], writes=["sinT"])
        P.dma(lamt, da_lam_d[j:j + 1, :].to_broadcast([128, 256]), reads=[], writes=["lamt"])
        l4 = lamt.rearrange("p (a b) -> p a b", a=4)
        _tt(P, "dve", tmp1[:, 0:64], l4[:, 0, :], l4[:, 1, :], ALU.mult, ["lamt"], ["tmp1"])
        _tt(P, "dve", tmp1[:, 64:128], l4[:, 2, :], l4[:, 3, :], ALU.mult, ["lamt"], ["tmp1"])
        P._add("dve", lambda e: e.tensor_reduce(out=lam[:, 0:2], in_=tmp1[:, 0:128].rearrange("p (a b) -> p a b", a=2), axis=AX.X,
                                                op=ALU.add), ["tmp1"], ["lam"])
        _act(P, lam[:, 0:2], lam[:, 0:2], AF.Exp, ["lam"], ["lam"])
        _tt(P, "dve", lam[:, 2:3], lam[:, 1:2], lam[:, 0:1], ALU.subtract, ["lam"], ["lam"])
        _ts(P, "dve", lam[:, 2:3], lam[:, 2:3], -lam_init, None, ALU.add, None, ["lam"], ["lam"])
        _memset(P, "pool", vda[:, :, :, 128:129], 1.0, ["vda"])
        for ci in range(8):
            dst = qda[:, ci, :] if ci < 4 else kda[:, ci - 4, :]
            dname = ("qda", ci) if ci < 4 else ("kda", ci - 4)
            if ci % 4 == 0:
                b0 = loadw(wv, 1552 + ci * 128, 512)
                b1 = loadw(wsw, ci * 128, 512)
            cc = (ci % 4) * 128
            for (t0, tn) in TG:
                tl = max(0, min(tn, TL - t0))
                for kc in range(KC):
                    _mm(P, ps[0][:, 0:tn], wst[:, b0, kc, cc:cc + 128], xT[:, kc, t0:t0 + tn], kc == 0, kc == KC - 1,
                        [("wst", b0)] + XTALL, [PSr(0)])
                if tl > 0:
                    for kc in range(KC):
                        _mm(P, ps[1][:, 0:tl], wst[:, b1, kc, cc:cc + 128], xT[:, kc, t0:t0 + tl], kc == 0, kc == KC - 1,
                            [("wst", b1)] + XTALL, [PSr(1)])
                    _tt(P, "dve", tmp1[:, 0:tl], ps[0][:, 0:tl], cosT[:, t0:t0 + tl], ALU.mult, [PSr(0), "cosT"], ["tmp1"])
                    _tt(P, "dve", tmp2[:, 0:tl], ps[1][:, 0:tl], sinT[:, t0:t0 + tl], ALU.mult, [PSr(1), "sinT"], ["tmp2"])
                    _tt(P, "dve", dst[:, t0:t0 + tl], tmp1[:, 0:tl], tmp2[:, 0:tl], ALU.add, ["tmp1", "tmp2"], [dname])
                if tl < tn:
                    _cp(P, "act", dst[:, t0 + tl:t0 + tn], ps[0][:, tl:tn], [PSr(0)], [dname])
        bdv = loadw(wv, 2576, 512)
        for blk in range(NB):
            for kc in range(KC):
                _mm(P, ps[2][:, :], xT[:, kc, blk * 128:(blk + 1) * 128], wst[:, bdv, kc, :], kc == 0, kc == KC - 1,
                    [("wst", bdv)] + XTALL, [PSr(2)])
            _cp(P, "act", vda[:, blk, :, 0:128], ps[2][:, :].rearrange("p (h d) -> p h d", h=4), [PSr(2)], ["vda"])
        P.barrier()
        AR.off = mark3
        wo = AR.alloc([KC, D], BF16)
        load_cast(wo, ev_w_out_d[j].rearrange("(k p) n -> p k n", p=128), "wo")
        for qb in qblocks:
            keys = list(range(NLB, NB)) + (list(range(NLB)) if qb < NLB else [])
            for h in range(4):
                for m in range(2):
                    qa = qda[m * 64:(m + 1) * 64, h, qb * 128:(qb + 1) * 128]
                    pso = 4 + m
                    for ki, kb in enumerate(keys):
                        ka = kda[m * 64:(m + 1) * 64, h, kb * 128:(kb + 1) * 128]
                        eb = nE[0] % 4
                        sb_ = 1 + nE[0] % 3
                        nE[0] += 1
                        attend_step(qa, ka, vda[:, kb, h, :], ps[pso][:, 0:129], PSr(pso), ki == 0, ki == len(keys) - 1,
                                    AF.Exp, 0.125, None, ("qda", h), ("kda", h), "vda", sb_, Et[:, eb, :], ("Et", eb))
                    def post_map(m=m, pso=pso, h=h):
                        P._add("dve", lambda e: e.reciprocal(out=r2[:, m:m + 1], in_=ps[pso][:, 128:129]), [PSr(pso)], ["r2"])
                        if m == 0:
                            _ts(P, "dve", o1, ps[4][:, 0:128], r2[:, 0:1], None, ALU.mult, None, [PSr(4), "r2"], ["o1"])
                        else:
                            _tt(P, "dve", r2[:, 1:2], r2[:, 1:2], lam[:, 2:3], ALU.mult, ["r2", "lam"], ["r2"])
                            _stt(P, "dve", hsum[:, h * 128:(h + 1) * 128], ps[5][:, 0:128], r2[:, 1:2], o1, ALU.mult, ALU.add,
                                 [PSr(5), "r2", "o1"], ["hsum"])
                    PIPE.post(post_map)

            def post_qb(qb=qb):
                headnorm(hsum, sq, s4, dag, 1.0 - lam_init, att[:, 512:1024], "att", None)
                _cp(P, "pool", att[:, 0:512], mlout[:, qb, :], ["og"], ["att"])
                out_proj_residual(qb, att, wo, after_block)
            PIPE.post(post_qb)
        PIPE.flush()
        P.barrier()

    def headnorm(hsum, sq, s4, gtile, mult, dst, dname, gate):
        h3 = hsum.rearrange("p (h d) -> p h d", h=4)
        _tt(P, "dve", sq, hsum, hsum, ALU.mult, ["hsum"], ["sq"])
        P._add("dve", lambda e: e.tensor_reduce(out=s4, in_=sq.rearrange("p (h d) -> p h d", h=4), axis=AX.X, op=ALU.add),
               ["sq"], ["s4"])
        _ts(P, "dve", s4, s4, 1.0 / 128, EPS, ALU.mult, ALU.add, ["s4"], ["s4"])
        _act(P, s4, s4, AF.Sqrt, ["s4"], ["s4"])
        P._add("dve", lambda e: e.reciprocal(out=s4, in_=s4), ["s4"], ["s4"])
        _tt(P, "dve", sq.rearrange("p (h d) -> p h d", h=4), h3, s4.unsqueeze(2).to_broadcast([128, 4, 128]), ALU.mult,
            ["hsum", "s4"], ["sq"])
        if gate is None:
            _stt(P, "dve", dst, sq, mult, gtile, ALU.mult, ALU.mult, ["sq"], [dname])
        else:
            _stt(P, "dve", sq, sq, mult, gtile, ALU.mult, ALU.mult, ["sq"], ["sq"])
            _tt(P, "dve", dst, sq, gate, ALU.mult, ["sq", "og"], [dname])

    def after_mixer(l):
        def f(blk, tile, tname):
            store_h(blk, tile, tname)
            norm_to_xT(blk, tile, tname, 1)
        return f

    def after_peer(l):
        last = (l == DEPTH - 1)

        def f(blk, tile, tname):
            if not last:
                store_h(blk, tile, tname)
            elif blk < NLB:
                final_norm(blk, tile, tname)
        return f

    fg = gates[:, 0, :]

    def final_norm(blk, tile, tname):
        ss = small[:, 2:3]
        rs = small[:, 3:4]
        _act(P, hC[:], tile[:], AF.Square, [tname], ["hC", "ss2"], accum_out=ss)
        _ts(P, "dve", rs, ss, 1.0 / D, EPS, ALU.mult, ALU.add, ["ss2"], ["rs2"])
        _act(P, rs, rs, AF.Sqrt, ["rs2"], ["rs2"])
        P._add("dve", lambda e: e.reciprocal(out=rs, in_=rs), ["rs2"], ["rs2"])
        _stt(P, "dve", hB[:], tile[:], rs, fg, ALU.mult, ALU.mult, [tname, "rs2", "gates"], ["hB"])
        P.dma(out_d[blk * 128:(blk + 1) * 128, :], hB[:], reads=["hB"], writes=["out"])

    import math
    for blk in range(NB):
        pass
    for l in range(DEPTH):
        need_ctx = l < DEPTH - 1
        modvec(l)
        for blk in range(NB):
            load_h(blk, hA, "hA")
            norm_to_xT(blk, hA, "hA", 0)
        P.barrier()
        if l % 2 == 0:
            even_mixer(l, l // 2, need_ctx, after_mixer(l), 0.8 - 0.6 * math.exp(-0.3 * l))
        else:
            odd_mixer(l, l // 2, need_ctx, after_mixer(l))
        pblocks = list(range(NLB)) + (list(range(NLB, NB)) if need_ctx else [])
        if l == DEPTH - 1:
            P.dma(fg, final_g_d.to_broadcast([128, D]), reads=["gates"], writes=["gates"])
        peer(l, pblocks, after_peer(l))
    P.finish(outputs=["out"])
    return nc, P


def _rope_tables(n_tok, grid_w=64, dim=64, theta=10000.0):
    rows = n_tok // grid_w
    r = np.repeat(np.arange(rows, dtype=np.float32), grid_w)
    col = np.tile(np.arange(grid_w, dtype=np.float32), rows)
    nf = dim // 4
    inv = (theta ** (-np.arange(nf, dtype=np.float32) / nf)).astype(np.float32)
    ang = np.concatenate([r[:, None] * inv, col[:, None] * inv], axis=-1).astype(np.float32)
    cos = np.cos(ang).astype(np.float32).T
    sin = np.sin(ang).astype(np.float32).T
    cos64 = np.concatenate([cos, cos], axis=0)
    sin64 = np.concatenate([-sin, sin], axis=0)
    return (np.ascontiguousarray(np.concatenate([cos64, cos64], axis=0)),
            np.ascontiguousarray(np.concatenate([sin64, sin64], axis=0)))


def _swap_cols(w):
    n = w.shape[-1]
    w4 = w.reshape(w.shape[:-1] + (n // 64, 2, 32))
    return np.ascontiguousarray(w4[..., ::-1, :].reshape(w.shape))


_PROG_CACHE = {}


def prepare_inputs(inp, NLB, NCB, DEPTH):
    f = lambda a: np.ascontiguousarray(np.asarray(a, dtype=np.float32))
    bsz = inp["x"].shape[0]
    NE = (DEPTH + 1) // 2
    NO = max(DEPTH // 2, 1)
    cos, sin = _rope_tables(NLB * 128)
    shared = {
        "mod_w": f(inp["mod_w"]), "mod_b": f(inp["mod_b"]), "norm1_g": f(inp["norm1_g"]), "norm2_g": f(inp["norm2_g"]),
        "ev_w_in": f(inp["ev_w_in"]), "ev_w_sw": _swap_cols(f(inp["ev_w_in"])[:, :, 1552:2576]),
        "ev_ml_conv_w": f(inp["ev_ml_conv_w"]), "ev_ml_conv_b": f(inp["ev_ml_conv_b"]), "ev_ml_gate_b": f(inp["ev_ml_gate_b"]),
        "ev_ml_norm_g": f(inp["ev_ml_norm_g"]), "ev_da_lam": f(inp["ev_da_lam"]).reshape(NE, 256),
        "ev_da_norm_g": f(inp["ev_da_norm_g"]), "ev_w_out": f(inp["ev_w_out"]),
        "pr_w_q": f(inp["pr_w_q"]),
        "pr_keysT": np.ascontiguousarray(f(inp["pr_keys"]).transpose(0, 1, 2, 4, 3).reshape(DEPTH, 16, 128, 128)),
        "pr_uT": np.ascontiguousarray(f(inp["pr_u"]).transpose(0, 2, 1)), "pr_v": f(inp["pr_v"]),
        "final_g": f(inp["final_g"]).reshape(1, -1), "rope_cos": cos, "rope_sin": sin,
    }
    if DEPTH // 2 > 0:
        shared.update({"od_w_in": f(inp["od_w_in"]), "od_w_sw": _swap_cols(f(inp["od_w_in"])[:, :, 0:1280]),
                       "od_sink": f(inp["od_sink"]), "od_w_out": f(inp["od_w_out"])})
    else:
        shared.update({"od_w_in": np.zeros((NO, 1024, 1536), np.float32), "od_w_sw": np.zeros((NO, 1024, 1280), np.float32),
                       "od_sink": np.zeros((NO, 16), np.float32), "od_w_out": np.zeros((NO, 1024, 1024), np.float32)})
    maps = []
    for b in range(bsz):
        m = dict(shared)
        m["x"] = f(inp["x"][b])
        m["ctx"] = f(inp["ctx"][b])
        m["c2"] = np.ascontiguousarray(np.stack([f(inp["c"][b]), f(inp["c_ctx"])], axis=0))
        maps.append(m)
    return maps


def kernel(**inputs):
    NLB, NCB, DEPTH = 16, 2, 4
    key = (NLB, NCB, DEPTH)
    if key not in _PROG_CACHE:
        _PROG_CACHE[key] = build_program(NLB, NCB, DEPTH)[0]
    nc = _PROG_CACHE[key]
    maps = prepare_inputs(inputs, NLB, NCB, DEPTH)
    res = run_bass_kernel_spmd(nc, maps, core_ids=list(range(len(maps))))
    return np.stack([np.asarray(r["out"], dtype=np.float32) for r in res.results], axis=0)
```
